# Optimizing a Trainium2 kernel written in Bass

```python
import jax, jax.numpy as jnp
from jax import lax
import numpy as np

D_MODEL = 2048
BATCH = 4
SEQ = 2048
DEPTH = 1

NSA_WIDTH = D_MODEL // 2
GLA_WIDTH = D_MODEL - NSA_WIDTH
MIX_WIDTH = NSA_WIDTH + GLA_WIDTH
NSA_HEAD_DIM = 64
NSA_HEADS = NSA_WIDTH // NSA_HEAD_DIM
NSA_KV_HEADS = 4
NSA_Q_PER_KV = NSA_HEADS // NSA_KV_HEADS
CMP_BLOCK = 32
CMP_STRIDE = 16
SEL_BLOCK = 64
N_SEL = 16
WINDOW = 512
N_BRANCH = 3
GLA_HEADS = 4
GLA_DV = GLA_WIDTH // GLA_HEADS
GLA_DK = GLA_DV // 2
GLA_GATE_RANK = 16
GLA_GATE_NORM = 16.0
GLA_CHUNK = 64
N_GROUPS = 4
EXPERTS_PER_GROUP = 8
N_EXPERTS = N_GROUPS * EXPERTS_PER_GROUP
TOP_K = 2
EXPERT_FF = D_MODEL // 4
MOE_BLOCK = 128
EPS = 1e-6
NEG = -1e30
FORCE = 1e30
IN_SPLITS = (NSA_HEADS * NSA_HEAD_DIM,
             2 * N_BRANCH * NSA_KV_HEADS * NSA_HEAD_DIM,
             N_BRANCH * NSA_HEADS,
             GLA_HEADS * GLA_DK,
             GLA_HEADS * GLA_DK,
             GLA_HEADS * GLA_DV,
             GLA_HEADS * GLA_DV,
             GLA_GATE_RANK)
IN_WIDTH = sum(IN_SPLITS)

kernel_name = 'hybrid_nsa_gla_hmoe_block'


def _rmsnorm(x, w):
    xf = x.astype(jnp.float32)
    xf = xf * lax.rsqrt(jnp.mean(xf * xf, axis=-1, keepdims=True) + EPS)
    return (xf * w.astype(jnp.float32)).astype(x.dtype)


def _alibi_slopes(n):
    return jnp.asarray(2.0 ** (-8.0 * np.arange(1, n + 1) / n), dtype=jnp.float32)


def _nsa(q, kv, gate_logits, pos_k, w1_k, w2_k, pos_v, w1_v, w2_v):
    B, S, _ = q.shape
    G, R, Dh = NSA_KV_HEADS, NSA_Q_PER_KV, NSA_HEAD_DIM
    f32 = jnp.float32
    out_dtype = q.dtype
    q = q.reshape(B, S, G, R, Dh).transpose(0, 2, 3, 1, 4) * (Dh ** -0.5)
    k_c, v_c, k_s, v_s, k_w, v_w = [a.reshape(B, S, G, Dh).transpose(0, 2, 1, 3)
                                    for a in jnp.split(kv, 6, axis=-1)]
    slopes = _alibi_slopes(NSA_HEADS).reshape(G, R)
    t = jnp.arange(S)

    n_cmp = (S - CMP_BLOCK) // CMP_STRIDE + 1
    starts = np.arange(n_cmp) * CMP_STRIDE
    tok = starts[:, None] + np.arange(CMP_BLOCK)[None, :]

    def compress(a, pos, w1, w2):
        blk = (a[:, :, tok, :] + pos).reshape(B, G, n_cmp, CMP_BLOCK * Dh)
        return jax.nn.gelu(blk @ w1) @ w2

    kc = compress(k_c, pos_k, w1_k, w2_k)
    vc = compress(v_c, pos_v, w1_v, w2_v)
    blk_end = jnp.asarray(starts + CMP_BLOCK - 1)
    blk_center = jnp.asarray(starts + (CMP_BLOCK - 1) / 2.0, dtype=f32)
    valid_c = blk_end[None, :] <= t[:, None]
    dist_c = t[:, None].astype(f32) - blk_center[None, :]
    s_c = jnp.einsum('bgrsd,bgnd->bgrsn', q, kc).astype(f32)
    s_c = jnp.where(valid_c, s_c - slopes[None, :, :, None, None] * dist_c, NEG)
    p_c = jnp.where(valid_c, jax.nn.softmax(s_c, axis=-1), 0.0)
    o_c = jnp.einsum('bgrsn,bgnd->bgrsd', p_c.astype(vc.dtype), vc)

    nb = S // SEL_BLOCK
    sel_start = np.arange(nb) * SEL_BLOCK
    overlap = ((starts[:, None] < sel_start[None, :] + SEL_BLOCK) &
               (starts[:, None] + CMP_BLOCK > sel_start[None, :])).astype(np.float32)
    imp = jnp.einsum('bgrsn,nj->bgsj', p_c, jnp.asarray(overlap))
    qblk = t // SEL_BLOCK
    j = jnp.arange(nb)
    forced = (j[None, :] == 0) | (j[None, :] == qblk[:, None]) | (j[None, :] == qblk[:, None] - 1)
    imp = jnp.where(forced, FORCE, jnp.where(j[None, :] <= qblk[:, None], imp, NEG))
    n_sel = min(N_SEL, nb)
    _, sel_idx = lax.top_k(imp, n_sel)

    QB = SEL_BLOCK
    nqb = S // QB
    ks_blk = k_s.reshape(B, G, nb, SEL_BLOCK, Dh)
    vs_blk = v_s.reshape(B, G, nb, SEL_BLOCK, Dh)
    kw_pad = jnp.pad(k_w, ((0, 0), (0, 0), (WINDOW, 0), (0, 0)))
    vw_pad = jnp.pad(v_w, ((0, 0), (0, 0), (WINDOW, 0), (0, 0)))
    q_blocks = jnp.moveaxis(q.reshape(B, G, R, nqb, QB, Dh), 3, 0)
    idx_blocks = jnp.moveaxis(sel_idx.reshape(B, G, nqb, QB, n_sel), 2, 0)
    gather = jax.vmap(jax.vmap(lambda blocks, ix: blocks[ix]))

    def block_step(args):
        i, qi, ix = args
        tq = i * QB + jnp.arange(QB)
        kg = gather(ks_blk, ix)
        vg = gather(vs_blk, ix)
        kpos = ix[..., None] * SEL_BLOCK + jnp.arange(SEL_BLOCK)
        dist = tq[None, None, :, None, None] - kpos
        s = jnp.einsum('bgrqd,bgqkld->bgrqkl', qi, kg).astype(f32)
        s = s - slopes[None, :, :, None, None, None] * dist[:, :, None].astype(f32)
        s = jnp.where((dist >= 0)[:, :, None], s, NEG)
        p = jax.nn.softmax(s, axis=(-2, -1))
        o_s = jnp.einsum('bgrqkl,bgqkld->bgrqd', p.astype(vg.dtype), vg)
        kw = lax.dynamic_slice_in_dim(kw_pad, i * QB, WINDOW + QB, axis=2)
        vw = lax.dynamic_slice_in_dim(vw_pad, i * QB, WINDOW + QB, axis=2)
        kpos_w = i * QB - WINDOW + jnp.arange(WINDOW + QB)
        dist_w = tq[:, None] - kpos_w[None, :]
        valid_w = (dist_w >= 0) & (dist_w < WINDOW) & (kpos_w[None, :] >= 0)
        s = jnp.einsum('bgrqd,bgkd->bgrqk', qi, kw).astype(f32)
        s = jnp.where(valid_w, s - slopes[None, :, :, None, None] * dist_w.astype(f32), NEG)
        p = jax.nn.softmax(s, axis=-1)
        o_w = jnp.einsum('bgrqk,bgkd->bgrqd', p.astype(vw.dtype), vw)
        return o_s, o_w

    o_s, o_w = lax.map(block_step, (jnp.arange(nqb), q_blocks, idx_blocks))
    o_s = jnp.moveaxis(o_s, 0, 3).reshape(B, G, R, S, Dh)
    o_w = jnp.moveaxis(o_w, 0, 3).reshape(B, G, R, S, Dh)

    g = jax.nn.sigmoid(gate_logits.astype(f32)).reshape(B, S, G, R, N_BRANCH).transpose(0, 2, 3, 1, 4)
    o = g[..., 0:1] * o_c + g[..., 1:2] * o_s + g[..., 2:3] * o_w
    return o.transpose(0, 3, 1, 2, 4).reshape(B, S, NSA_HEADS * Dh).astype(out_dtype)


def _gla(q, k, v, out_gate, gate_lr, w_gate2, b_gate, norm_w):
    B, S, _ = q.shape
    H, Dk, Dv, C = GLA_HEADS, GLA_DK, GLA_DV, GLA_CHUNK
    f32 = jnp.float32
    out_dtype = q.dtype
    log_a = jax.nn.log_sigmoid((gate_lr @ w_gate2 + b_gate).astype(f32)) / GLA_GATE_NORM
    nc = S // C

    def chunks(a, d):
        return jnp.moveaxis(a.astype(f32).reshape(B, nc, C, H, d), 1, 0).transpose(0, 1, 3, 2, 4)

    qs = chunks(q, Dk) * (Dk ** -0.5)
    ks = chunks(k, Dk)
    vs = chunks(v, Dv)
    gs = chunks(log_a, Dk)
    causal = jnp.tril(jnp.ones((C, C), dtype=bool))

    def step(state, inp):
        qc, kc, vc, gc = inp
        b = jnp.cumsum(gc, axis=2)
        o_inter = jnp.einsum('bhik,bhkv->bhiv', qc * jnp.exp(b), state)
        decay = jnp.exp(jnp.where(causal[:, :, None], b[:, :, :, None, :] - b[:, :, None, :, :], -jnp.inf))
        attn = jnp.einsum('bhik,bhjk,bhijk->bhij', qc, kc, decay)
        o_intra = jnp.einsum('bhij,bhjv->bhiv', attn, vc)
        b_last = b[:, :, -1, :]
        state = jnp.exp(b_last)[..., None] * state + jnp.einsum(
            'bhjk,bhjv->bhkv', kc * jnp.exp(b_last[:, :, None, :] - b), vc)
        return state, o_inter + o_intra

    state0 = jnp.zeros((B, H, Dk, Dv), f32)
    _, o = lax.scan(step, state0, (qs, ks, vs, gs))
    o = o.transpose(1, 0, 3, 2, 4).reshape(B, S, H, Dv)
    o = o * lax.rsqrt(jnp.mean(o * o, axis=-1, keepdims=True) + EPS) * norm_w.astype(f32)
    o = o.reshape(B, S, H * Dv) * jax.nn.silu(out_gate.astype(f32))
    return o.astype(out_dtype)


def _moe(h, w_rg, b_rg, w_re, b_re, w_gate, w_up, w_down):
    B, S, D = h.shape
    N = B * S
    NK = N * TOP_K
    f32 = jnp.float32
    xt = h.reshape(N, D)
    grp_prob = jax.nn.softmax((xt @ w_rg + b_rg).astype(f32), axis=-1)
    grp_p, grp = lax.top_k(grp_prob, 1)
    exp_logits = (xt @ w_re + b_re).astype(f32).reshape(N, N_GROUPS, EXPERTS_PER_GROUP)
    exp_logits = jnp.take_along_axis(exp_logits, grp[:, :, None], axis=1)[:, 0]
    top_v, top_i = lax.top_k(exp_logits, TOP_K)
    weight = grp_p * jax.nn.softmax(top_v, axis=-1)
    expert = grp * EXPERTS_PER_GROUP + top_i
    flat_e = expert.reshape(-1)
    flat_w = weight.reshape(-1)
    flat_tok = jnp.arange(NK) // TOP_K
    counts = jnp.zeros((N_EXPERTS,), jnp.int32).at[flat_e].add(1)
    padded = (counts + MOE_BLOCK - 1) // MOE_BLOCK * MOE_BLOCK
    pad_end = jnp.cumsum(padded)
    pad_start = pad_end - padded
    start = jnp.cumsum(counts) - counts
    order = jnp.argsort(flat_e, stable=True)
    e_sorted = flat_e[order]
    dest = pad_start[e_sorted] + jnp.arange(NK) - start[e_sorted]
    cap = ((NK + N_EXPERTS * (MOE_BLOCK - 1) + MOE_BLOCK - 1) // MOE_BLOCK) * MOE_BLOCK
    n_blk = cap // MOE_BLOCK
    slot_tok = jnp.full((cap,), N, jnp.int32).at[dest].set(flat_tok[order])
    slot_w = jnp.zeros((cap,), f32).at[dest].set(flat_w[order])
    blk_expert = jnp.minimum(jnp.searchsorted(pad_end, jnp.arange(n_blk) * MOE_BLOCK, side='right'),
                             N_EXPERTS - 1)
    x_slots = jnp.concatenate([xt, jnp.zeros((1, D), xt.dtype)], axis=0)[slot_tok]
    x_slots = x_slots.reshape(n_blk, MOE_BLOCK, D)

    def expert_ffn(args):
        xb, e = args
        return (jax.nn.silu(xb @ w_gate[e]) * (xb @ w_up[e])) @ w_down[e]

    y = lax.map(expert_ffn, (x_slots, blk_expert)).reshape(cap, D)
    y = y * slot_w[:, None].astype(y.dtype)
    y = jnp.zeros((N + 1, D), y.dtype).at[slot_tok].add(y)[:N]
    return y.reshape(B, S, D)


def setup_inputs(seed: int = 0) -> dict:
    key = jax.random.key(seed)
    ks = jax.random.split(key, 32)
    L, D, Dh = DEPTH, D_MODEL, NSA_HEAD_DIM

    def nrm(k, shape, scale):
        return jax.random.normal(k, shape, jnp.float32) * scale

    return {
        'x': nrm(ks[0], (BATCH, SEQ, D), 1.0),
        'c': nrm(ks[1], (BATCH, D), 1.0),
        'w_ada': nrm(ks[2], (L, D, 6 * D), 0.5 * D ** -0.5),
        'b_ada': nrm(ks[3], (L, 6 * D), 0.02),
        'norm1_w': 1.0 + nrm(ks[4], (L, D), 0.02),
        'w_in': nrm(ks[5], (L, D, IN_WIDTH), D ** -0.5),
        'cmp_pos_k': nrm(ks[6], (L, CMP_BLOCK, Dh), 0.1),
        'cmp_w1_k': nrm(ks[7], (L, CMP_BLOCK * Dh, Dh), (CMP_BLOCK * Dh) ** -0.5),
        'cmp_w2_k': nrm(ks[8], (L, Dh, Dh), Dh ** -0.5),
        'cmp_pos_v': nrm(ks[9], (L, CMP_BLOCK, Dh), 0.1),
        'cmp_w1_v': nrm(ks[10], (L, CMP_BLOCK * Dh, Dh), (CMP_BLOCK * Dh) ** -0.5),
        'cmp_w2_v': nrm(ks[11], (L, Dh, Dh), Dh ** -0.5),
        'gla_w_gate2': nrm(ks[12], (L, GLA_GATE_RANK, GLA_HEADS * GLA_DK), GLA_GATE_RANK ** -0.5),
        'gla_b_gate': nrm(ks[13], (L, GLA_HEADS * GLA_DK), 0.1),
        'gla_norm_w': 1.0 + nrm(ks[14], (L, GLA_DV), 0.02),
        'w_out': nrm(ks[15], (L, MIX_WIDTH, D), MIX_WIDTH ** -0.5),
        'norm2_w': 1.0 + nrm(ks[16], (L, D), 0.02),
        'w_router_group': nrm(ks[17], (L, D, N_GROUPS), D ** -0.5),
        'b_router_group': nrm(ks[18], (L, N_GROUPS), 0.01),
        'w_router_expert': nrm(ks[19], (L, D, N_EXPERTS), D ** -0.5),
        'b_router_expert': nrm(ks[20], (L, N_EXPERTS), 0.01),
        'w_expert_gate': nrm(ks[21], (L, N_EXPERTS, D, EXPERT_FF), D ** -0.5),
        'w_expert_up': nrm(ks[22], (L, N_EXPERTS, D, EXPERT_FF), D ** -0.5),
        'w_expert_down': nrm(ks[23], (L, N_EXPERTS, EXPERT_FF, D), EXPERT_FF ** -0.5),
        'norm_f_w': 1.0 + nrm(ks[24], (D,), 0.02),
    }


def reference(x, c, w_ada, b_ada, norm1_w, w_in, cmp_pos_k, cmp_w1_k, cmp_w2_k, cmp_pos_v,
              cmp_w1_v, cmp_w2_v, gla_w_gate2, gla_b_gate, gla_norm_w, w_out, norm2_w,
              w_router_group, b_router_group, w_router_expert, b_router_expert,
              w_expert_gate, w_expert_up, w_expert_down, norm_f_w):
    offsets = np.cumsum(IN_SPLITS)[:-1].tolist()
    for l in range(DEPTH):
        mod = jax.nn.silu(c) @ w_ada[l] + b_ada[l]
        sh1, sc1, g1, sh2, sc2, g2 = [m[:, None, :] for m in jnp.split(mod, 6, axis=-1)]
        h = _rmsnorm(x, norm1_w[l]) * (1.0 + sc1) + sh1
        proj = h @ w_in[l]
        nsa_q, nsa_kv, nsa_g, gla_q, gla_k, gla_v, gla_og, gla_lr = jnp.split(proj, offsets, axis=-1)
        o_nsa = _nsa(nsa_q, nsa_kv, nsa_g, cmp_pos_k[l], cmp_w1_k[l], cmp_w2_k[l],
                     cmp_pos_v[l], cmp_w1_v[l], cmp_w2_v[l])
        o_gla = _gla(gla_q, gla_k, gla_v, gla_og, gla_lr, gla_w_gate2[l], gla_b_gate[l], gla_norm_w[l])
        mix = jnp.concatenate([o_nsa, o_gla], axis=-1)
        x = x + g1 * (mix @ w_out[l])
        h = _rmsnorm(x, norm2_w[l]) * (1.0 + sc2) + sh2
        x = x + g2 * _moe(h, w_router_group[l], b_router_group[l], w_router_expert[l],
                          b_router_expert[l], w_expert_gate[l], w_expert_up[l], w_expert_down[l])
    return _rmsnorm(x, norm_f_w)
```

```python
import numpy as np
import ml_dtypes
import concourse.bass as bass
import concourse.mybir as mybir
from concourse.bass_utils import run_bass_kernel_spmd

F32 = mybir.dt.float32
BF16 = mybir.dt.bfloat16
U32 = mybir.dt.uint32
AF = mybir.ActivationFunctionType
ALU = mybir.AluOpType
AX = mybir.AxisListType
NPBF = ml_dtypes.bfloat16

D = 2048
SEQ = 2048
NCTX = 2048
NOWN = 1024
KC = 16
EPS = 1e-6
NEGB = -30000.0
IN_WIDTH = 5696
OFF_Q, OFF_KV, OFF_G = 0, 1024, 2560
OFF_GQ, OFF_GK, OFF_GV, OFF_GO, OFF_LR = 2608, 3120, 3632, 4656, 5680
MOE_NBLK = 1
SAME_ENGINE_SYNC = True


class Buf:
    __slots__ = ("name", "w", "r", "dsem", "excl")

    def __init__(self, name, excl=False):
        self.name = name
        self.excl = excl
        self.w = {}
        self.r = {}
        self.dsem = None


class Prog:
    ENGS = ("sp", "act", "pool", "dve", "pe")

    def __init__(self, nc):
        self.nc = nc
        self.ops = {e: [] for e in self.ENGS}
        self.sems = {}
        self.cnt = {}
        self.seen = {e: {} for e in self.ENGS}
        self.pending = {e: {} for e in self.ENGS}
        for e in self.ENGS:
            self._mksem("E_" + e)
        self.ndsem = 0

    def _mksem(self, key):
        self.sems[key] = self.nc.alloc_semaphore(key)
        self.cnt[key] = 0
        return key

    def _need(self, eng, waits, key, val):
        if key == "E_" + eng and (eng == "pe" or not SAME_ENGINE_SYNC):
            return
        if self.seen[eng].get(key, 0) >= val:
            return
        if waits.get(key, 0) < val:
            waits[key] = val

    def _collect(self, eng, reads, writes, pwrites, extra=None):
        waits = dict(self.pending[eng])
        self.pending[eng] = {}
        for b in reads:
            for k, v in b.w.items():
                self._need(eng, waits, k, v)
            if b.excl:
                for k, v in b.r.items():
                    if k != "E_" + eng:
                        self._need(eng, waits, k, v)
        for b in writes:
            for k, v in b.w.items():
                self._need(eng, waits, k, v)
            for k, v in b.r.items():
                self._need(eng, waits, k, v)
        for b in pwrites:
            for k, v in b.r.items():
                self._need(eng, waits, k, v)
        if extra is not None:
            self._need(eng, waits, extra[0], extra[1])
        for k, v in waits.items():
            if self.seen[eng].get(k, 0) < v:
                self.seen[eng][k] = v
        return waits

    def _commit(self, key, val, reads, writes, pwrites):
        for b in reads:
            b.r[key] = val
        for b in writes:
            b.w = {key: val}
            b.r = {}
        for b in pwrites:
            b.w[key] = val

    def op(self, eng, fn, reads=(), writes=(), pwrites=()):
        waits = self._collect(eng, reads, writes, pwrites)
        key = "E_" + eng
        self.cnt[key] += 1
        self._commit(key, self.cnt[key], reads, writes, pwrites)
        self.ops[eng].append((list(waits.items()), fn, (key, 1)))

    def dma(self, q, fns, reads=(), writes=(), pwrites=(), owner=None):
        if callable(fns):
            fns = [(q, fns)]
        if owner is None:
            owner = writes[0] if writes else (pwrites[0] if pwrites else reads[0])
        if owner.dsem is None:
            self.ndsem += 1
            owner.dsem = self._mksem("D%d" % self.ndsem)
        dkey = owner.dsem
        prev = self.cnt[dkey]
        for (qq, fn) in fns:
            waits = self._collect(qq, reads, writes, pwrites, extra=(dkey, prev) if prev else None)
            self.ops[qq].append((list(waits.items()), fn, (dkey, 16)))
        self.cnt[dkey] += 16 * len(fns)
        self._commit(dkey, self.cnt[dkey], reads, writes, pwrites)

    def barrier(self):
        for e in self.ENGS:
            for k, v in self.cnt.items():
                if not v:
                    continue
                if k == "E_" + e and (e == "pe" or not SAME_ENGINE_SYNC):
                    continue
                if self.seen[e].get(k, 0) < v and self.pending[e].get(k, 0) < v:
                    self.pending[e][k] = v

    def finish(self):
        self.barrier()
        for e in self.ENGS:
            waits = self.pending[e]
            self.pending[e] = {}
            for k, v in waits.items():
                self.seen[e][k] = v
            if waits:
                self.ops[e].append((list(waits.items()), None, None))

    def replay(self):
        nc = self.nc
        P = self

        def run(ename, eng):
            for waits, fn, inc in P.ops[ename]:
                for k, v in waits:
                    eng.wait_ge(P.sems[k], v)
                if fn is None:
                    continue
                ins = fn(eng)
                if inc is not None:
                    ins.then_inc(P.sems[inc[0]], inc[1])

        with nc.Block() as block:
            @block.sync
            def _(e):
                run("sp", e)

            @block.scalar
            def _(e):
                run("act", e)

            @block.gpsimd
            def _(e):
                run("pool", e)

            @block.vector
            def _(e):
                run("dve", e)

            @block.tensor
            def _(e):
                run("pe", e)


class Arena:
    def __init__(self, nc):
        self.nc = nc
        self.lo = (nc.sbuf_base + 63) // 64 * 64
        self.hi = nc.sbuf_top
        self.cur = self.lo
        self.n = 0
        self.peak = self.lo

    def alloc(self, name, shape, dtype):
        esz = 2 if dtype == BF16 else 4
        per = int(np.prod(shape[1:])) * esz
        off = (self.cur + 63) // 64 * 64
        assert off + per <= self.hi, "SBUF overflow %s need %d at %d (hi %d)" % (name, per, off, self.hi)
        self.n += 1
        t = self.nc.alloc_sbuf_tensor_at("%s_%d" % (name, self.n), list(shape), dtype, offset=off)
        self.cur = off + per
        self.peak = max(self.peak, self.cur)
        self.last_off = off
        return t

    def alloc_at(self, name, shape, dtype, off):
        self.n += 1
        return self.nc.alloc_sbuf_tensor_at("%s_%d" % (name, self.n), list(shape), dtype, offset=off)

    def mark(self):
        return self.cur

    def release(self, m):
        self.cur = m


def _slopes():
    return (2.0 ** (-8.0 * np.arange(1, 17) / 16)).astype(np.float32)


def make_consts(s):
    c = {}
    t = np.arange(NCTX)
    first_real = 0 if s == 1 else 1024
    ka = np.zeros((36, NCTX), np.float32)
    ka[np.arange(NCTX) // 64, np.arange(NCTX)] = 1.0
    ka[32] = 64.0 * (t // 64)
    ka[33] = t % 64
    ka[34] = np.where(t < first_real, NEGB, 0.0)
    ka[35] = 1.0
    c["kaug"] = ka.astype(NPBF)
    n = np.arange(128)
    kc = np.zeros((36, 128), np.float32)
    kc[32] = 16.0 * n
    kc[34] = np.where((16 * n < first_real) | (n >= 127), NEGB, 0.0)
    kc[35] = 1.0
    c["kcaug"] = kc.astype(NPBF)
    sl = _slopes()
    tq = (1024 + np.arange(NOWN)).astype(np.float32)
    qa = np.zeros((16, 4, NOWN), np.float32)
    for h in range(16):
        slb = np.float32(sl[h]).astype(NPBF).astype(np.float32)
        qa[h, 0] = slb
        qa[h, 1] = slb
        qa[h, 2] = 1.0
        qa[h, 3] = -slb * tq
    c["qaug"] = qa.astype(NPBF)
    k = np.arange(128)[:, None]
    q = np.arange(512)[None, :]
    cb = np.zeros((8, 128, 512), np.float32)
    for i, o in enumerate(range(-4, 0)):
        cb[i] = np.where(q - (128 * o + k) >= 512, NEGB, 0.0)
    for i, o in enumerate(range(0, 4)):
        cb[4 + i] = np.where((128 * o + k) > q, NEGB, 0.0)
    c["cb"] = cb.astype(NPBF)
    cbc = np.zeros((2, 128, 512), np.float32)
    for ch in range(2):
        tqq = 1024 + 512 * ch + q
        cbc[ch] = np.where(16 * k + 31 > tqq, NEGB, 0.0)
    c["cbc"] = cbc.astype(NPBF)
    Fm = np.zeros((128, 8, 32), np.float32)
    Um = np.zeros((128, 8, 32), np.float32)
    fb = first_real // 64
    for i in range(8):
        tt = 1024 + 128 * i + np.arange(128)
        qb = (tt // 64)[:, None]
        j = np.arange(32)[None, :]
        forced = ((j == fb) | (j == qb) | (j == qb - 1)) & (j >= fb) & (j <= qb)
        Fm[:, i, :] = np.where(forced, 1e30, -3e38)
        Um[:, i, :] = np.where((j >= fb) & (j <= qb), 3e38, -1e30)
    c["selF"] = Fm
    c["selU"] = Um
    ov = np.zeros((128, 33), np.float32)
    for nn in range(127):
        for jj in range(32):
            if (16 * nn < 64 * jj + 64) and (16 * nn + 32 > 64 * jj):
                ov[nn, jj] = 1.0
        ov[nn, 32] = 1.0
    c["ovaug"] = ov.astype(NPBF)
    jj = np.arange(128)[:, None]
    ii = np.arange(128)[None, :]
    same = (jj // 64) == (ii // 64)
    c["glamask"] = (same & (jj <= ii)).astype(np.float32)
    L = np.zeros((128, 130), np.float32)
    L[:, :128] = np.where(same & (jj <= ii), -1.0 / 16, 0.0)
    L[:64, 128] = -1.0 / 16
    L[64:, 129] = -1.0 / 16
    c["lmat"] = L
    c["umat"] = np.where(same & (jj > ii), -1.0 / 16, 0.0).astype(np.float32)
    c["ident_f"] = np.eye(128, dtype=np.float32)
    c["ident_b"] = np.eye(128, dtype=np.float32).astype(NPBF)
    e64 = np.zeros((64, 192), np.float32)
    e64[np.arange(64), 64 + np.arange(64)] = 1.0
    c["e64"] = e64.astype(NPBF)
    c["oh12"] = np.eye(12, dtype=np.float32).reshape(1, 144)
    s12 = np.zeros((12, 12, 64), np.float32)
    for i in range(12):
        s12[i, i, :] = 1.0
    c["sel12"] = s12.astype(NPBF)
    sp_ = np.zeros((12, 6, 128), np.float32)
    for pair in range(2):
        for br in range(3):
            sp_[(2 * pair) * 3 + br, pair * 3 + br, 0:64] = 1.0
            sp_[(2 * pair + 1) * 3 + br, pair * 3 + br, 64:128] = 1.0
    c["selpair"] = sp_.astype(NPBF)
    c["ones_b"] = np.ones((128, 128), np.float32).astype(NPBF)
    c["ones_f"] = np.ones((128, 128), np.float32)
    c["iota"] = np.tile(np.arange(128, dtype=np.float32)[None, :], (128, 1))
    c["lstrict"] = (jj < ii).astype(np.float32).astype(NPBF)
    c["pvalid"] = np.full((128, 1), 1.0 if s == 1 else 0.0, np.float32)
    return c


CONST_DT = {"kaug": BF16, "kcaug": BF16, "qaug": BF16, "cb": BF16, "cbc": BF16, "ovaug": BF16,
            "ident_b": BF16, "e64": BF16, "sel12": BF16, "selpair": BF16, "ones_b": BF16, "lstrict": BF16}


def build(stage=99, debug=()):
    nc = bass.Bass("TRN2", target_bir_lowering=False)
    P = Prog(nc)
    A = Arena(nc)
    DR = {}
    OUTS = {}

    def din(name, shape, dt=F32):
        DR[name] = nc.dram_tensor(name, list(shape), dt, kind="ExternalInput").ap()
        return DR[name]

    def dout(name, shape, dt=F32):
        OUTS[name] = nc.dram_tensor(name, list(shape), dt, kind="ExternalOutput").ap()
        return OUTS[name]

    def MM(out, lhsT, rhs, start=True, stop=True, R=(), W=(), PW=()):
        P.op("pe", lambda e: e.matmul(out, lhsT, rhs, start=start, stop=stop), R, W, PW)

    def TR(out, in_, ident, R=(), W=(), PW=()):
        P.op("pe", lambda e: e.transpose(out, in_, ident), R, W, PW)

    def ACT(out, in_, func, R=(), W=(), PW=(), scale=None, bias=None, accum=None):
        kw = {}
        if scale is not None:
            kw["scale"] = scale
        if bias is not None:
            kw["bias"] = bias
        if accum is not None:
            kw["accum_out"] = accum
        P.op("act", lambda e: e.activation(out, in_, func, **kw), R, W, PW)

    def TT(eng, out, a, b, op, R=(), W=(), PW=()):
        P.op(eng, lambda e: e.tensor_tensor(out, a, b, op), R, W, PW)

    def TS(eng, out, a, s1, s2, op0, op1=None, R=(), W=(), PW=()):
        if op1 is None:
            P.op(eng, lambda e: e.tensor_scalar(out, a, s1, s2, op0), R, W, PW)
        else:
            P.op(eng, lambda e: e.tensor_scalar(out, a, s1, s2, op0, op1), R, W, PW)

    def STT(out, a, s, b, op0, op1, R=(), W=(), PW=()):
        P.op("dve", lambda e: e.scalar_tensor_tensor(out, a, s, b, op0, op1), R, W, PW)

    def CP(eng, out, in_, R=(), W=(), PW=()):
        if eng == "act":
            P.op("act", lambda e: e.copy(out, in_), R, W, PW)
        else:
            P.op(eng, lambda e: e.tensor_copy(out, in_), R, W, PW)

    def MS(eng, ap, val, W=(), PW=()):
        P.op(eng, lambda e: e.memset(ap, val), (), W, PW)

    def RECIP(out, in_, R=(), W=(), PW=()):
        P.op("dve", lambda e: e.reciprocal(out, in_), R, W, PW)

    def DMA(q, out, in_, R=(), W=(), PW=(), owner=None, **kw):
        P.dma(q, lambda e: e.dma_start(out=out, in_=in_, **kw), R, W, PW, owner)

    dbg_n = [0]

    def DBG(name, ap, shape, R, dt=F32):
        if name not in debug:
            return
        o = dout("dbg_" + name, shape, dt)
        DMA("sp", o, ap, R=R, W=[Buf("dbg_" + name)])

    PS = [nc.alloc_psum_tensor("psb%d" % i, [128, 512], F32) for i in range(8)]
    bPS = [Buf("ps%d" % i, excl=True) for i in range(8)]

    xctx = din("xctx", [NCTX, D])
    cT_d = din("cT", [128, 16])
    w_ada = din("w_ada", [D, 6 * D])
    b_ada = din("b_ada", [24, 512])
    n1w_d = din("n1w", [128, 16])
    w_ada_v = w_ada.rearrange("(kc p) n -> p kc n", p=128)
    consts_np = make_consts(0)
    CD = {}
    for k, v in consts_np.items():
        CD[k] = din("c_" + k, list(v.shape), CONST_DT.get(k, F32))

    b_const = Buf("consts")
    C = {}
    cfn = []

    def cload(name, shape, src_ap, dt=None):
        dt = dt or CONST_DT.get(name, F32)
        t = A.alloc("c_" + name, shape, dt)
        C[name] = t
        cfn.append(("sp", lambda e, t=t, s=src_ap: e.dma_start(out=t[:], in_=s)))
        return t

    cload("ident_f", [128, 128], CD["ident_f"])
    cload("ident_b", [128, 128], CD["ident_b"])
    cload("ones_b", [128, 128], CD["ones_b"])
    cload("ones_f", [128, 128], CD["ones_f"])
    cload("cT", [128, 16], cT_d, F32)
    cload("n1w", [128, 16], n1w_d, F32)
    w_in = din("w_in", [D, IN_WIDTH])
    w_in_v = w_in.rearrange("(kc p) n -> p kc n", p=128)
    w2aug_d = din("w2aug", [17, 512])
    gnw_d = din("gnw", [128, 2])
    cload("gnw", [128, 2], gnw_d, F32)
    cload("glamask", [128, 128], CD["glamask"])
    cload("lmat", [128, 130], CD["lmat"])
    cload("umat", [128, 128], CD["umat"])
    cload("e64", [64, 192], CD["e64"])
    cload("ovaug", [128, 33], CD["ovaug"])
    cload("iota", [128, 128], CD["iota"])
    cload("lstrict", [128, 128], CD["lstrict"])
    cload("pvalid", [128, 1], CD["pvalid"])
    P.dma("sp", cfn, writes=[b_const])

    NRING = 2
    ring = [A.alloc("ring%d" % i, [128, 8192], BF16) for i in range(NRING)]
    bring = [Buf("ring%d" % i) for i in range(NRING)]
    rstate = [0]

    def ring_next():
        i = rstate[0] % NRING
        rstate[0] += 1
        return ring[i], bring[i]

    def v16(t, n=512):
        return t[:, 0:16 * n].rearrange("p (a b) -> p a b", a=16)

    cols1 = A.alloc("cols1", [128, 2, 16], F32)
    b_cols1 = Buf("cols1")
    scl = A.alloc("scl", [128, 16], F32)
    b_scl = Buf("scl")
    adacols = A.alloc("adacols", [128, 64], F32)
    b_adac = Buf("adacols")
    brow = [A.alloc("brow%d" % i, [1, 512], BF16) for i in range(2)]
    b_brow = [Buf("brow%d" % i) for i in range(2)]
    mixT = A.alloc("mixT", [128, KC, NOWN], BF16)
    off_mix = A.last_off
    b_mix = [Buf("mix%d" % i) for i in range(16)]
    off_mixT = A.lo
    m_big = A.mark()
    hT = A.alloc("hT", [128, KC, NCTX], BF16)
    b_hT = [Buf("hT%d" % t) for t in range(16)]
    m_after_hT = A.mark()

    ACT(scl[:], C["cT"][:], AF.Silu, R=[b_const], W=[b_scl])
    screp_state = {}

    def make_screp():
        t = A.alloc("sc_rep", [128, 16, 128], BF16)
        b = Buf("screp")
        for kc in range(16):
            TS("dve", t[:, kc, :], C["ones_b"][:], scl[:, kc:kc + 1], None, ALU.mult,
               R=[b_const, b_scl], PW=[b])
        screp_state["t"] = t
        screp_state["b"] = b

    make_screp()
    m_tmp = A.mark()
    ada_n = [0]

    def ada_block(j, dst, dstbuf, psi, excl_dst=False):
        slot, bslot = ring_next()
        sv = v16(slot)
        DMA("pool", sv, w_ada_v[:, :, j * 512:(j + 1) * 512], W=[bslot])
        bi = ada_n[0] % 2
        ada_n[0] += 1
        DMA("pool", brow[bi][0:1, :], b_ada[j:j + 1, :], W=[b_brow[bi]])
        for kc in range(16):
            MM(PS[psi][:, :], screp_state["t"][:, kc, :], sv[:, kc, :], start=(kc == 0), stop=False,
               R=[bslot, screp_state["b"]], W=[bPS[psi]] if kc == 0 else (), PW=() if kc == 0 else [bPS[psi]])
        MM(PS[psi][:, :], C["ones_b"][0:1, :], brow[bi][0:1, :], start=False, stop=True,
           R=[b_const, b_brow[bi]], PW=[bPS[psi]])
        CP("act", dst, PS[psi][:, :], R=[bPS[psi]], PW=[dstbuf])

    dgt = [None]

    def ada_rebuild(v, dst, dstbuf):
        if dgt[0] is None:
            dgt[0] = ([A.alloc("dg%d" % i, [128, 128], F32) for i in range(2)], [Buf("dg%d" % i) for i in range(2)])
        dg, b_dg = dgt[0]
        for q4 in range(4):
            psi = q4 % 2
            for c4 in range(4):
                cc = 4 * q4 + c4
                di = cc % 2
                TS("dve", dg[di][:, :], C["ident_f"][:, :], adacols[:, 16 * v + cc:16 * v + cc + 1], None, ALU.mult,
                   R=[b_const, b_adac], W=[b_dg[di]])
                MM(PS[psi][:, c4 * 128:(c4 + 1) * 128], C["ones_f"][:, :], dg[di][:, :], R=[b_const, b_dg[di]],
                   W=[bPS[psi]] if c4 == 0 else (), PW=() if c4 == 0 else [bPS[psi]])
            CP("act", dst[:, q4 * 512:(q4 + 1) * 512], PS[psi][:, :], R=[bPS[psi]], PW=[dstbuf])

    bc1 = A.alloc("bc1", [128, 2, 2048], F32)
    b_bc1 = Buf("bc1")
    for j in range(8):
        ada_block(j, bc1[:, j // 4, (j % 4) * 512:(j % 4 + 1) * 512], b_bc1, j % 2)
    DBG("bc1", bc1[0:1, :, :], [1, 2, 2048], [b_bc1])
    dtmp = A.alloc("dtmp", [128, 16, 128], F32)
    b_dtmp = Buf("dtmp")
    col16 = A.alloc("col16", [128, 2, 16], F32)
    b_col16 = Buf("col16")
    idb = C["ident_f"][:, :].unsqueeze(1).broadcast_to([128, 16, 128])
    for which in range(2):
        TT("dve", dtmp[:, :, :], bc1[:, which, :].rearrange("p (a b) -> p a b", a=16), idb, ALU.mult,
           R=[b_bc1, b_const], W=[b_dtmp])
        P.op("dve", lambda e, w=which: e.tensor_reduce(col16[:, w, :], dtmp[:, :, :], AX.X, ALU.add),
             [b_dtmp], (), [b_col16])
    CP("dve", cols1[:, 0, :], col16[:, 0, :], R=[b_col16], PW=[b_cols1])
    STT(cols1[:, 1, :], col16[:, 1, :], 1.0, C["n1w"][:], ALU.add, ALU.mult,
        R=[b_col16, b_const], PW=[b_cols1])
    DBG("cols1", cols1[:, :, :], [128, 2, 16], [b_cols1])
    P.barrier()
    A.release(m_tmp)
    if stage <= 0:
        P.finish()
        P.replay()
        return nc, OUTS

    xt = [A.alloc("xt%d" % i, [128, D], F32) for i in range(2)]
    b_xt = [Buf("xt%d" % i) for i in range(2)]
    xs = [A.alloc("xs%d" % i, [128, D], BF16) for i in range(2)]
    b_xs = [Buf("xs%d" % i) for i in range(2)]
    junk = A.alloc("junk", [128, D], BF16)
    b_junk = Buf("junk")
    st1 = A.alloc("st1", [128, 16, 4], F32)
    b_st1 = [Buf("st1_%d" % t) for t in range(16)]
    import os
    PH1_T = int(os.environ.get('PH1_T', '16'))
    PH1_M = int(os.environ.get('PH1_M', '9'))
    adat = [A.alloc("adat%d" % i, [128, 512], F32) for i in range(2)]
    b_adat = [Buf("adat%d" % i) for i in range(2)]
    dt4 = A.alloc("dt4", [128, 4, 128], F32)
    b_dt4 = Buf("dt4")
    idb4 = C["ident_f"][:, :].unsqueeze(1).broadcast_to([128, 4, 128])
    for t in range(PH1_T):
        i2 = t % 2
        ada_block(8 + t, adat[i2][:, :], b_adat[i2], 4 + i2, excl_dst=True)
        TT("dve", dt4[:, :, :], adat[i2][:, :].rearrange("p (a b) -> p a b", a=4), idb4, ALU.mult,
           R=[b_adat[i2], b_const], W=[b_dt4])
        P.op("dve", lambda e, t=t: e.tensor_reduce(adacols[:, 4 * t:4 * t + 4], dt4[:, :, :], AX.X, ALU.add),
             [b_dt4], (), [b_adac])
        DMA("sp", xt[i2][:], xctx[t * 128:(t + 1) * 128, :], W=[b_xt[i2]])
        ACT(junk[:], xt[i2][:], AF.Square, R=[b_xt[i2]], W=[b_junk, b_st1[t]], accum=st1[:, t, 0:1])
        if PH1_M < 1:
            continue
        ACT(st1[:, t, 1:2], st1[:, t, 0:1], AF.Sqrt, R=[b_st1[t]], PW=[b_st1[t]], scale=1.0 / D, bias=EPS)
        if PH1_M < 2:
            continue
        RECIP(st1[:, t, 2:3], st1[:, t, 1:2], R=[b_st1[t]], PW=[b_st1[t]])
        TS("dve", xs[i2][:], xt[i2][:], st1[:, t, 2:3], None, ALU.mult, R=[b_xt[i2], b_st1[t]], W=[b_xs[i2]])
        if PH1_M < 3:
            continue
        for half in range(2):
            psi = 2 + half
            pv = PS[psi][:, :].bitcast(BF16)
            for k8 in range(8):
                kc = half * 8 + k8
                TR(pv[:, k8 * 128:(k8 + 1) * 128], xs[i2][:, kc * 128:(kc + 1) * 128], C["ident_b"][:],
                   R=[b_xs[i2], b_const], W=[bPS[psi]] if k8 == 0 else (), PW=() if k8 == 0 else [bPS[psi]])
            if PH1_M < 4:
                continue
            for k8 in range(8):
                kc = half * 8 + k8
                dst = hT[:, kc, t * 128:(t + 1) * 128]
                src = pv[:, k8 * 128:(k8 + 1) * 128]
                if half == 0:
                    ACT(dst, src, AF.Identity, R=[bPS[psi], b_cols1], PW=[b_hT[t]],
                        scale=cols1[:, 1, kc:kc + 1], bias=cols1[:, 0, kc:kc + 1])
                else:
                    TS("dve", dst, src, cols1[:, 1, kc:kc + 1], cols1[:, 0, kc:kc + 1], ALU.mult, ALU.add,
                       R=[bPS[psi], b_cols1], PW=[b_hT[t]])
    if "hT" in debug:
        o = dout("dbg_hT", [128, KC, NCTX], BF16)
        bd = Buf("dbg_hT")
        for kc in range(KC):
            DMA("sp", o[:, kc, :], hT[:, kc, :], R=b_hT, PW=[bd])
    P.barrier()
    m_tmp = m_after_hT
    A.release(m_tmp)
    if stage <= 1:
        P.finish()
        P.replay()
        return nc, OUTS

    DK = 128
    lrT = A.alloc("lrT", [32, NCTX], BF16)
    b_lrT = Buf("lrT")
    w2a = A.alloc("w2a", [32, 512], BF16)
    b_w2a = Buf("w2a")
    l_tok = A.alloc("l_tok", [128, 16, 128], F32)
    b_ltok = [Buf("ltok%d" % t) for t in range(16)]
    etmp = A.alloc("etmp", [128, 128], F32)
    b_etmp = Buf("etmp")
    ebT = A.alloc("ebT", [128, 512], F32)
    enbT = A.alloc("enbT", [128, 512], F32)
    b_eb = [Buf("eb%d" % i) for i in range(4)]
    ehat = A.alloc("ehat", [128, 4, 128], F32)
    b_ehat = [Buf("ehat%d" % i) for i in range(4)]
    dec = A.alloc("dec", [128, 16, 2], F32)
    b_dec = [Buf("dec%d" % t) for t in range(16)]
    khat = [A.alloc("khat%d" % i, [128, 4, 128], BF16) for i in range(2)]
    vtok = [A.alloc("vtok%d" % i, [128, 4, 256], BF16) for i in range(2)]
    b_kv = [[Buf("kv%d_%d" % (i, j)) for j in range(4)] for i in range(2)]
    qtT = A.alloc("qtT", [128, NOWN], F32)
    ktT = A.alloc("ktT", [128, NOWN], F32)
    b_qk = [Buf("qk%d" % i) for i in range(2)]
    ogT = A.alloc("ogT", [128, 2, NOWN], BF16)
    b_og = [Buf("og%d" % i) for i in range(2)]
    Sst = [A.alloc("S%d" % i, [128, 256], F32) for i in range(3)]
    b_S = [Buf("S%d" % i) for i in range(3)]
    ATm = A.alloc("ATm", [128, 128], BF16)
    b_ATm = Buf("ATm")
    oT_sb = A.alloc("oT_sb", [128, 2, 512], F32)
    b_oT = [Buf("oT%d" % i) for i in range(4)]
    sqb = A.alloc("sqb", [128, 2, 512], BF16)
    b_sq = Buf("sq")
    rsd = A.alloc("rsd", [128, 512], F32)
    b_rsd = Buf("rsd")
    t1b = A.alloc("t1b", [128, 512], F32)
    b_t1 = Buf("t1")

    MS("pool", lrT[:, :], 1.0, W=[b_lrT])
    DMA("pool", w2a[0:17, :], w2aug_d, W=[b_w2a])
    (Z_, G_, KV_, PA_, PB_, U_, A_, O2_) = range(8)
    projb = [PA_, PB_]
    pj = [0]

    def proj_fm(wv, c0, ncols, tok0, ntok, bslot, tiles):
        psi = projb[pj[0] % 2]
        pj[0] += 1
        for kc in range(16):
            MM(PS[psi][0:ncols, 0:ntok], wv[:, kc, c0:c0 + ncols], hT[:, kc, tok0:tok0 + ntok],
               start=(kc == 0), stop=(kc == 15), R=[bslot] + [b_hT[t] for t in tiles],
               W=[bPS[psi]] if kc == 0 else (), PW=() if kc == 0 else [bPS[psi]])
        return psi

    for hg in range(4):
        slotA, bA = ring_next()
        svA = v16(slotA)
        P.dma("pool", [("pool", lambda e, hg=hg: e.dma_start(out=svA[:, :, 0:128], in_=w_in_v[:, :, OFF_GQ + hg * 128:OFF_GQ + (hg + 1) * 128])),
                       ("pool", lambda e, hg=hg: e.dma_start(out=svA[:, :, 128:256], in_=w_in_v[:, :, OFF_GK + hg * 128:OFF_GK + (hg + 1) * 128])),
                       ("pool", lambda e, hg=hg: e.dma_start(out=svA[:, :, 256:512], in_=w_in_v[:, :, OFF_GV + hg * 256:OFF_GV + (hg + 1) * 256]))],
              writes=[bA])
        slotB, bB = ring_next()
        svB = v16(slotB)
        fnsB = [("pool", lambda e, hg=hg: e.dma_start(out=svB[:, :, 0:256], in_=w_in_v[:, :, OFF_GO + hg * 256:OFF_GO + (hg + 1) * 256]))]
        if hg == 0:
            fnsB.append(("pool", lambda e: e.dma_start(out=svB[:, :, 256:272], in_=w_in_v[:, :, OFF_LR:OFF_LR + 16])))
        P.dma("pool", fnsB, writes=[bB])
        if hg == 0:
            for cj in range(4):
                psi = proj_fm(svB, 256, 16, cj * 512, 512, bB, range(4 * cj, 4 * cj + 4))
                CP("act", lrT[0:16, cj * 512:(cj + 1) * 512], PS[psi][0:16, 0:512], R=[bPS[psi]], PW=[b_lrT])
        MS("dve", Sst[0][:, :], 0.0, W=[b_S[0]])
        scur = 0
        for cj in range(4):
            own = cj >= 2
            par = cj % 2
            oc0 = (cj - 2) * 512
            for tl in range(4):
                t = 4 * cj + tl
                MM(PS[Z_][:, 0:128], lrT[0:17, t * 128:(t + 1) * 128], w2a[0:17, hg * 128:(hg + 1) * 128],
                   R=[b_lrT, b_w2a], W=[bPS[Z_]])
                KVb = [KV_, PA_][t % 2]
                for kc in range(16):
                    MM(PS[KVb][:, 0:384], hT[:, kc, t * 128:(t + 1) * 128], svA[:, kc, 128:512],
                       start=(kc == 0), stop=(kc == 15), R=[bA, b_hT[t]],
                       W=[bPS[KVb]] if kc == 0 else (), PW=() if kc == 0 else [bPS[KVb]])
                ACT(etmp[:, :], PS[Z_][:, 0:128], AF.Exp, R=[bPS[Z_]], W=[b_etmp], scale=-1.0)
                ACT(l_tok[:, t, :], etmp[:, :], AF.Ln, R=[b_etmp], W=[b_ltok[t]], bias=1.0)
                MM(PS[G_][:, 0:130], l_tok[:, t, :], C["lmat"][:, 0:130], R=[b_ltok[t], b_const], W=[bPS[G_]])
                MM(PS[G_][:, 256:384], C["umat"][:, :], l_tok[:, t, :], R=[b_ltok[t], b_const], PW=[bPS[G_]])
                if own:
                    ACT(ebT[:, tl * 128:(tl + 1) * 128], PS[G_][:, 0:128], AF.Exp, R=[bPS[G_]], W=[b_eb[tl]])
                    ACT(enbT[:, tl * 128:(tl + 1) * 128], PS[G_][:, 0:128], AF.Exp, R=[bPS[G_]], PW=[b_eb[tl]], scale=-1.0)
                ACT(dec[:, t, :], PS[G_][:, 128:130], AF.Exp, R=[bPS[G_]], W=[b_dec[t]])
                ACT(ehat[:, tl, :], PS[G_][:, 256:384], AF.Exp, R=[bPS[G_]], W=[b_ehat[tl]])
                if own:
                    TT("dve", khat[par][:, tl, :], PS[KVb][:, 0:128], ehat[:, tl, :], ALU.mult,
                       R=[bPS[KVb], b_ehat[tl]], W=[b_kv[par][tl]])
                else:
                    STT(khat[par][:, tl, :], PS[KVb][:, 0:128], C["pvalid"][:, 0:1], ehat[:, tl, :], ALU.mult, ALU.mult,
                        R=[bPS[KVb], b_ehat[tl], b_const], W=[b_kv[par][tl]])
                CP("dve", vtok[par][:, tl, :], PS[KVb][:, 128:384], R=[bPS[KVb]], PW=[b_kv[par][tl]])
            if own:
                tiles = range(4 * cj, 4 * cj + 4)
                psi = proj_fm(svA, 0, 128, cj * 512, 512, bA, tiles)
                STT(qtT[:, oc0:oc0 + 512], PS[psi][:, 0:512], float(DK) ** -0.5, ebT[:, :], ALU.mult, ALU.mult,
                    R=[bPS[psi]] + b_eb, W=[b_qk[cj - 2]])
                psi = proj_fm(svA, 128, 128, cj * 512, 512, bA, tiles)
                TT("dve", ktT[:, oc0:oc0 + 512], PS[psi][:, 0:512], enbT[:, :], ALU.mult,
                   R=[bPS[psi]] + b_eb, PW=[b_qk[cj - 2]])
                for half in range(2):
                    psi = proj_fm(svB, half * 128, 128, cj * 512, 512, bB, tiles)
                    ACT(ogT[:, half, oc0:oc0 + 512], PS[psi][:, 0:512], AF.Silu, R=[bPS[psi]],
                        W=[b_og[cj - 2]] if half == 0 else (), PW=() if half == 0 else [b_og[cj - 2]])
            for tl in range(4):
                t = 4 * cj + tl
                MM(PS[U_][:, 0:256], khat[par][0:64, tl, :], vtok[par][0:64, tl, :], R=[b_kv[par][tl]], W=[bPS[U_]])
                MM(PS[A_][:, 256:512], khat[par][64:128, tl, :], vtok[par][64:128, tl, :], R=[b_kv[par][tl]], W=[bPS[A_]])
                s0, s1, s2 = scur, (scur + 1) % 3, (scur + 2) % 3
                if own:
                    tc0 = oc0 + tl * 128
                    MM(PS[A_][:, 0:128], ktT[:, tc0:tc0 + 128], qtT[:, tc0:tc0 + 128], R=[b_qk[cj - 2]], PW=[bPS[A_]])
                    TT("dve", ATm[:, :], PS[A_][:, 0:128], C["glamask"][:, :], ALU.mult, R=[bPS[A_], b_const], W=[b_ATm])
                STT(Sst[s1][:, :], Sst[s0][:, :], dec[:, t, 0:1], PS[U_][:, 0:256], ALU.mult, ALU.add,
                    R=[b_S[s0], b_dec[t], bPS[U_]], W=[b_S[s1]])
                STT(Sst[s2][:, :], Sst[s1][:, :], dec[:, t, 1:2], PS[A_][:, 256:512], ALU.mult, ALU.add,
                    R=[b_S[s1], b_dec[t], bPS[A_]], W=[b_S[s2]])
                if own:
                    for half in range(2):
                        hs = slice(half * 128, (half + 1) * 128)
                        MM(PS[O2_][:, hs], vtok[par][:, tl, hs], ATm[:, :], start=True, stop=False,
                           R=[b_kv[par][tl], b_ATm], W=[bPS[O2_]] if half == 0 else (), PW=() if half == 0 else [bPS[O2_]])
                        MM(PS[O2_][:, half * 128:half * 128 + 64], Sst[s0][:, hs], qtT[:, tc0:tc0 + 64],
                           start=False, stop=False, R=[b_S[s0], b_qk[cj - 2]], PW=[bPS[O2_]])
                        MM(PS[O2_][:, half * 128 + 64:half * 128 + 128], Sst[s1][:, hs], qtT[:, tc0 + 64:tc0 + 128],
                           start=False, stop=True, R=[b_S[s1], b_qk[cj - 2]], PW=[bPS[O2_]])
                    CP("act", oT_sb[:, :, tl * 128:(tl + 1) * 128],
                       PS[O2_][:, 0:256].rearrange("p (a b) -> p a b", a=2), R=[bPS[O2_]], W=[b_oT[tl]])
                scur = s2
            if own:
                ACT(sqb[:, :, :], oT_sb[:, :, :], AF.Square, R=b_oT, W=[b_sq])
                MM(PS[Z_][:, 0:512], C["ones_b"][:, :], sqb[:, 0, :], start=True, stop=False, R=[b_const, b_sq], W=[bPS[Z_]])
                MM(PS[Z_][:, 0:512], C["ones_b"][:, :], sqb[:, 1, :], start=False, stop=True, R=[b_const, b_sq], PW=[bPS[Z_]])
                ACT(rsd[:, :], PS[Z_][:, 0:512], AF.Sqrt, R=[bPS[Z_]], W=[b_rsd], scale=1.0 / 256, bias=EPS)
                RECIP(rsd[:, :], rsd[:, :], R=[b_rsd], W=[b_rsd])
                for half in range(2):
                    TT("dve", t1b[:, :], oT_sb[:, half, :], rsd[:, :], ALU.mult, R=b_oT + [b_rsd], W=[b_t1])
                    mc = 8 + 2 * hg + half
                    STT(mixT[:, mc, oc0:oc0 + 512], t1b[:, :], C["gnw"][:, half:half + 1], ogT[:, half, oc0:oc0 + 512],
                        ALU.mult, ALU.mult, R=[b_t1, b_const, b_og[cj - 2]], PW=[b_mix[mc]])
    print("SBUF KiB used end of GLA", (A.cur - A.lo) / 1024.0)
    if "mixgla" in debug:
        o = dout("dbg_mixgla", [128, 8, NOWN], BF16)
        bd = Buf("dbg_mixgla")
        for kc in range(8):
            DMA("sp", o[:, kc, :], mixT[:, 8 + kc, :], R=b_mix[8:16], PW=[bd])
    P.barrier()
    A.release(m_tmp)
    if stage <= 2:
        P.finish()
        P.replay()
        return nc, OUTS

    cw1_d = din("cw1", [128, 32, 64])
    cw2_d = din("cw2", [128, 64])
    cpos_d = din("cposT", [128, 32])
    b_c2 = Buf("consts2")
    cb = A.alloc("cb", [128, 8, 512], BF16)
    cbc = A.alloc("cbc", [128, 2, 512], BF16)
    selF = A.alloc("selF", [128, 8, 32], F32)
    selU = A.alloc("selU", [128, 8, 32], F32)
    ksaT = A.alloc("ksaT", [100, NCTX], BF16)
    kwaT = A.alloc("kwaT", [100, NCTX], BF16)
    kcaT = A.alloc("kcaT", [100, 128], BF16)
    b_ksa, b_kwa, b_kca = Buf("ksaT"), Buf("kwaT"), Buf("kcaT")
    vcaug = A.alloc("vcaug", [128, 76], BF16)
    b_vca = Buf("vcaug")
    qaT = [A.alloc("qaT%d" % r, [100, NOWN], BF16) for r in range(4)]
    b_qa = [Buf("qaT%d" % r) for r in range(4)]
    b_qsel = [Buf("qsel%d" % r) for r in range(4)]
    kvcT = A.alloc("kvcT", [128, NCTX], BF16)
    b_kvc = Buf("kvcT")
    vsw = A.alloc("vsw", [128, 16, 2, 76], BF16)
    b_vsw = [Buf("vsw%d" % t) for t in range(16)]
    gT = A.alloc("gT", [76, NOWN], BF16)
    b_gT = Buf("gT")
    cw1 = A.alloc("cw1", [128, 32, 64], BF16)
    cw2 = A.alloc("cw2", [128, 64], BF16)
    cposT = A.alloc("cposT", [128, 32], BF16)
    cbias = A.alloc("cbias", [128, 1], F32)
    b_cbias = Buf("cbias")
    gu = A.alloc("gu", [128, 128], F32)
    gq = A.alloc("gq", [128, 128], F32)
    gl = A.alloc("gl", [128, 128], BF16)
    b_gu, b_gq, b_gl = Buf("gu"), Buf("gq"), Buf("gl")
    NPT = 4
    pTb = [A.alloc("pT%d" % i, [128, 512], BF16) for i in range(NPT)]
    b_pT = [Buf("pT%d" % i) for i in range(NPT)]
    numb = A.alloc("numb", [128, 6, 512], BF16)
    b_numb = [Buf("numb%d" % i) for i in range(6)]
    dacc = A.alloc("dacc", [76, 512], F32)
    b_dacc = Buf("dacc")
    Dsb = A.alloc("Dsb", [76, 512], F32)
    b_Dsb = Buf("Dsb")
    fT = A.alloc("fT", [76, 512], BF16)
    b_fT = Buf("fT")
    ohcol = A.alloc("ohcol", [76, 12], F32)
    sel12p = A.alloc("sel12p", [76, 768], BF16)
    tmul = [A.alloc("tmul%d" % i, [128, 512], BF16) for i in range(2)]
    b_tmul = [Buf("tmul%d" % i) for i in range(2)]
    impt = A.alloc("impt", [128, 4, 32], F32)
    b_imp = [Buf("imp%d" % i) for i in range(4)]
    sm = A.alloc("sm", [128, 4, 8], F32)
    b_sm = [Buf("sm%d" % i) for i in range(4)]
    imp2 = A.alloc("imp2", [128, 32], F32)
    imp3 = A.alloc("imp3", [128, 32], F32)
    m8a = A.alloc("m8a", [128, 8], F32)
    m8b = A.alloc("m8b", [128, 8], F32)
    b_sel = Buf("selscratch")
    selb4 = A.alloc("selb4", [128, 4, 128], BF16)
    b_selb4 = [Buf("selb%d" % i) for i in range(4)]

    fns = [("sp", lambda e: e.dma_start(out=cb[:, :, :], in_=CD["cb"].rearrange("i k q -> k i q"))),
           ("sp", lambda e: e.dma_start(out=cbc[:, :, :], in_=CD["cbc"].rearrange("i k q -> k i q"))),
           ("sp", lambda e: e.dma_start(out=selF[:, :, :], in_=CD["selF"])),
           ("sp", lambda e: e.dma_start(out=selU[:, :, :], in_=CD["selU"])),
           ("sp", lambda e: e.dma_start(out=ksaT[64:100, :], in_=CD["kaug"])),
           ("sp", lambda e: e.dma_start(out=kwaT[96:100, :], in_=CD["kaug"][32:36, :])),
           ("sp", lambda e: e.dma_start(out=kcaT[96:100, :], in_=CD["kcaug"][32:36, :])),
           ("sp", lambda e: e.dma_start(out=ohcol[64:76, :], in_=CD["oh12"].rearrange("a (b c) -> (a b) c", c=12))),
           ("sp", lambda e: e.dma_start(out=sel12p[64:76, :], in_=CD["selpair"].rearrange("a b c -> a (b c)"))),
           ]
    P.dma("sp", fns, writes=[b_c2])
    b_c2p = Buf("consts2p")
    P.dma("pool", [("pool", lambda e: e.dma_start(out=cw1[:, :, :], in_=cw1_d)),
                   ("pool", lambda e: e.dma_start(out=cw2[:, :], in_=cw2_d)),
                   ("pool", lambda e: e.dma_start(out=cposT[:, :], in_=cpos_d))], writes=[b_c2p])
    MS("pool", kwaT[64:96, :], 0.0, PW=[b_kwa])
    MS("pool", kcaT[64:96, :], 0.0, PW=[b_kca])
    MS("pool", kcaT[0:64, :], 0.0, PW=[b_kca])
    MS("pool", vcaug[:, :], 1.0, W=[b_vca])
    MS("pool", vsw[:, :, :, :], 1.0, W=b_vsw)
    MS("pool", selb4[:, :, :], 0.0, W=b_selb4)
    for r in range(4):
        MS("pool", qaT[r][64:96, :], 0.0, W=[b_qsel[r]])
    (S0_, S1_, AC0_, AC1_, D_, S2_, M_, I_) = range(8)
    F_ = I_
    projb[:] = [S0_, S1_, AC0_, AC1_]

    def proj_fm4(wv, c0, ncols, tok0, ntok, bslot, tiles, prow=0):
        psi = projb[pj[0] % 4]
        pj[0] += 1
        for kc in range(16):
            MM(PS[psi][prow:prow + ncols, 0:ntok], wv[:, kc, c0:c0 + ncols], hT[:, kc, tok0:tok0 + ntok],
               start=(kc == 0), stop=(kc == 15), R=[bslot] + [b_hT[t] for t in tiles],
               W=[bPS[psi]] if kc == 0 else (), PW=() if kc == 0 else [bPS[psi]])
        return psi

    for l in range(32):
        MM(PS[S0_][0:64, 0:1], cw1[0:64, l, :], cposT[0:64, l:l + 1], start=(l == 0), stop=(l == 31),
           R=[b_c2p], W=[bPS[S0_]] if l == 0 else (), PW=() if l == 0 else [bPS[S0_]])
        MM(PS[S1_][64:128, 0:1], cw1[64:128, l, :], cposT[64:128, l:l + 1], start=(l == 0), stop=(l == 31),
           R=[b_c2p], W=[bPS[S1_]] if l == 0 else (), PW=() if l == 0 else [bPS[S1_]])
    CP("act", cbias[0:64, :], PS[S0_][0:64, 0:1], R=[bPS[S0_]], PW=[b_cbias])
    CP("act", cbias[64:128, :], PS[S1_][64:128, 0:1], R=[bPS[S1_]], PW=[b_cbias])
    kvc_v = kvcT[:, :].rearrange("p (n s) -> p s n", s=16)
    GC = 2.0 * 0.7978845608028654

    pt_i = [0]
    sc_i = [0]
    acc_i = [0]
    den_i = [0]
    tm_i = [0]
    fb_i = [0]

    for g in range(4):
        slotA, bA = ring_next()
        svA = v16(slotA)
        P.dma("pool", [("pool", lambda e, g=g: e.dma_start(out=svA[:, :, 0:256], in_=w_in_v[:, :, OFF_Q + g * 256:OFF_Q + (g + 1) * 256])),
                       ("pool", lambda e, g=g: e.dma_start(out=svA[:, :, 256:268], in_=w_in_v[:, :, OFF_G + g * 12:OFF_G + (g + 1) * 12]))],
              writes=[bA])
        slotB, bB = ring_next()
        svB = v16(slotB)
        order = [0, 1, 2, 4, 3, 5]
        P.dma("pool", [("pool", lambda e, g=g, i=i, br=br: e.dma_start(
            out=svB[:, :, i * 64:(i + 1) * 64],
            in_=w_in_v[:, :, OFF_KV + br * 256 + g * 64:OFF_KV + br * 256 + (g + 1) * 64])) for i, br in enumerate(order)],
              writes=[bB])
        P.dma("sp", [("sp", lambda e, g=g, r=r: e.dma_start(out=qaT[r][96:100, :], in_=CD["qaug"][4 * g + r, :, :])) for r in range(4)],
              pwrites=b_qa)
        for pair in range(2):
            for c in range(2):
                psi = proj_fm4(svA, pair * 128, 128, 1024 + c * 512, 512, bA, range(8 + 4 * c, 12 + 4 * c))
                for rr in range(2):
                    r = 2 * pair + rr
                    P.op("act", lambda e, r=r, rr=rr, c=c, psi=psi: e.mul(qaT[r][0:64, c * 512:(c + 1) * 512],
                                                                          PS[psi][rr * 64:(rr + 1) * 64, 0:512], 0.125),
                         [bPS[psi]], (), [b_qa[r]])
        for c in range(2):
            psi = proj_fm4(svA, 256, 12, 1024 + c * 512, 512, bA, range(8 + 4 * c, 12 + 4 * c), prow=64)
            ACT(gT[64:76, c * 512:(c + 1) * 512], PS[psi][64:76, 0:512], AF.Sigmoid, R=[bPS[psi]], PW=[b_gT])
        for cj in range(4):
            tiles = range(4 * cj, 4 * cj + 4)
            psi = proj_fm4(svB, 0, 128, cj * 512, 512, bB, tiles)
            CP("dve", kvcT[:, cj * 512:(cj + 1) * 512], PS[psi][:, 0:512], R=[bPS[psi]], PW=[b_kvc])
            psi = proj_fm4(svB, 128, 128, cj * 512, 512, bB, tiles)
            CP("act", ksaT[0:64, cj * 512:(cj + 1) * 512], PS[psi][0:64, 0:512], R=[bPS[psi]], PW=[b_ksa])
            CP("act", kwaT[0:64, cj * 512:(cj + 1) * 512], PS[psi][64:128, 0:512], R=[bPS[psi]], PW=[b_kwa])
        for t in range(16):
            psi = projb[pj[0] % 4]
            pj[0] += 1
            for kc in range(16):
                MM(PS[psi][:, 0:128], hT[:, kc, t * 128:(t + 1) * 128], svB[:, kc, 256:384],
                   start=(kc == 0), stop=(kc == 15), R=[bB, b_hT[t]],
                   W=[bPS[psi]] if kc == 0 else (), PW=() if kc == 0 else [bPS[psi]])
            CP("act" if t % 2 else "dve", vsw[:, t, :, 0:64], PS[psi][:, 0:128].rearrange("p (a b) -> p a b", a=2),
               R=[bPS[psi]], PW=[b_vsw[t]])
        for l in range(32):
            MM(PS[S0_][0:64, 0:127], cw1[0:64, l, :], kvc_v[0:64, l % 16, l // 16:l // 16 + 127],
               start=(l == 0), stop=(l == 31), R=[b_c2p, b_kvc], W=[bPS[S0_]] if l == 0 else (), PW=() if l == 0 else [bPS[S0_]])
            MM(PS[S1_][64:128, 0:127], cw1[64:128, l, :], kvc_v[64:128, l % 16, l // 16:l // 16 + 127],
               start=(l == 0), stop=(l == 31), R=[b_c2p, b_kvc], W=[bPS[S1_]] if l == 0 else (), PW=() if l == 0 else [bPS[S1_]])
        ACT(gu[0:64, 0:127], PS[S0_][0:64, 0:127], AF.Identity, R=[bPS[S0_], b_cbias], W=[b_gu], bias=cbias[0:64, 0:1])
        ACT(gu[64:128, 0:127], PS[S1_][64:128, 0:127], AF.Identity, R=[bPS[S1_], b_cbias], PW=[b_gu], bias=cbias[64:128, 0:1])
        ACT(gq[:, 0:127], gu[:, 0:127], AF.Square, R=[b_gu], W=[b_gq])
        TS("dve", gq[:, 0:127], gq[:, 0:127], 0.044715, 1.0, ALU.mult, ALU.add, R=[b_gq], W=[b_gq])
        TT("dve", gq[:, 0:127], gq[:, 0:127], gu[:, 0:127], ALU.mult, R=[b_gq, b_gu], W=[b_gq])
        ACT(gq[:, 0:127], gq[:, 0:127], AF.Sigmoid, R=[b_gq], W=[b_gq], scale=GC)
        TT("dve", gl[:, 0:127], gq[:, 0:127], gu[:, 0:127], ALU.mult, R=[b_gq, b_gu], W=[b_gl])
        MM(PS[S0_][0:64, 0:127], cw2[0:64, :], gl[0:64, 0:127], R=[b_c2p, b_gl], W=[bPS[S0_]])
        MM(PS[S1_][0:127, 0:64], gl[64:128, 0:127], cw2[64:128, :], R=[b_c2p, b_gl], W=[bPS[S1_]])
        CP("act", kcaT[0:64, 0:127], PS[S0_][0:64, 0:127], R=[bPS[S0_]], W=[b_kca])
        CP("dve", vcaug[0:127, 0:64], PS[S1_][0:127, 0:64], R=[bPS[S1_]], PW=[b_vca])
        if "cmpkv" in debug and g == 0:
            o = dout("dbg_kca", [64, 128], BF16)
            DMA("sp", o, kcaT[0:64, :], R=[b_kca], W=[Buf("dbg_kca")])
            o = dout("dbg_vca", [128, 65], BF16)
            DMA("sp", o, vcaug[:, :], R=[b_vca], W=[Buf("dbg_vca")])

        def finalize(ai, r, br, first, last):
            idx = r * 3 + br
            k6 = (r // 2) * 3 + br
            hp = (r % 2) * 64
            CP("act", numb[hp:hp + 64, k6, :], PS[ai][0:64, :], R=[bPS[ai]], PW=[b_numb[k6]])
            if first:
                TS("dve", dacc[64:76, :], PS[ai][64:76, :], ohcol[64:76, idx:idx + 1], None, ALU.mult,
                   R=[bPS[ai], b_c2], W=[b_dacc])
            else:
                STT(dacc[64:76, :], PS[ai][64:76, :], ohcol[64:76, idx:idx + 1], dacc[64:76, :], ALU.mult, ALU.add,
                    R=[bPS[ai], b_c2, b_dacc], W=[b_dacc])

        def branch(r, br, ktiles, c):
            ai = [AC0_, AC1_][acc_i[0] % 2]
            acc_i[0] += 1
            n = len(ktiles)
            pend = None
            for i, (kap, kb, bias, vap, vb, K, qlo, qhi, blo, bhi) in enumerate(ktiles):
                si = [S0_, S1_, S2_][sc_i[0] % 3]
                sc_i[0] += 1
                MM(PS[si][0:K, qlo:qhi], kap, qaT[r][0:100, c * 512 + qlo:c * 512 + qhi], start=True, stop=(bias is None),
                   R=kb + [b_qa[r], b_qsel[r], b_c2], W=[bPS[si]])
                if bias is not None:
                    MM(PS[si][0:K, blo:bhi], C["ident_b"][0:K, 0:K], bias[0:K, blo:bhi], start=False, stop=True,
                       R=[b_const, b_c2], PW=[bPS[si]])
                if pend is not None:
                    pend()
                pi = pt_i[0] % NPT
                pt_i[0] += 1
                ACT(pTb[pi][0:K, qlo:qhi], PS[si][0:K, qlo:qhi], AF.Exp, R=[bPS[si]], W=[b_pT[pi]])

                def pv(i=i, pi=pi, vap=vap, vb=vb, K=K, qlo=qlo, qhi=qhi):
                    MM(PS[ai][0:76, qlo:qhi], vap, pTb[pi][0:K, qlo:qhi], start=(i == 0), stop=(i == n - 1),
                       R=vb + [b_pT[pi]], W=[bPS[ai]] if i == 0 else (), PW=() if i == 0 else [bPS[ai]])
                pend = pv
                if br == 0:
                    pend()
                    pend = None
                    for qt in range(4):
                        MM(PS[I_][:, qt * 64:qt * 64 + 33], pTb[pi][0:127, qt * 128:(qt + 1) * 128], C["ovaug"][0:127, 0:33],
                           R=[b_pT[pi], b_const], W=[bPS[I_]] if qt == 0 else (), PW=() if qt == 0 else [bPS[I_]])
                    for qt in range(4):
                        TS("dve", sm[:, qt, 0:1], PS[I_][:, qt * 64 + 32:qt * 64 + 33], 1e-30, None, ALU.max,
                           R=[bPS[I_]], W=[b_sm[qt]])
                        RECIP(sm[:, qt, 1:2], sm[:, qt, 0:1], R=[b_sm[qt]], PW=[b_sm[qt]])
                        if r == 0:
                            TS("dve", impt[:, qt, :], PS[I_][:, qt * 64:qt * 64 + 32], sm[:, qt, 1:2], None, ALU.mult,
                               R=[bPS[I_], b_sm[qt]], W=[b_imp[qt]])
                        else:
                            STT(impt[:, qt, :], PS[I_][:, qt * 64:qt * 64 + 32], sm[:, qt, 1:2], impt[:, qt, :], ALU.mult, ALU.add,
                                R=[bPS[I_], b_sm[qt], b_imp[qt]], W=[b_imp[qt]])
            if pend is not None:
                pend()
            return ai

        for c in range(2):
            q0 = 8 + 4 * c
            nfin = [0]
            for r in range(4):
                ai = branch(r, 0, [(kcaT[0:100, 0:127], [b_kca], cbc[:, c, :], vcaug[0:127, 0:76], [b_vca], 127, 0, 512, 0, 512)], c)
                finalize(ai, r, 0, nfin[0] == 0, False)
                nfin[0] += 1
            for qt in range(4):
                qi = 4 * c + qt
                TT("dve", imp2[:, :], impt[:, qt, :], selF[:, qi, :], ALU.max, R=[b_imp[qt], b_c2], W=[b_sel])
                TT("dve", imp2[:, :], imp2[:, :], selU[:, qi, :], ALU.min, R=[b_c2], W=[b_sel])
                P.op("dve", lambda e: e.max(m8a[:, :], imp2[:, :]), [], [b_sel])
                P.op("dve", lambda e: e.match_replace(imp3[:, :], m8a[:, :], imp2[:, :], -3.0e38), [], [b_sel])
                P.op("dve", lambda e: e.max(m8b[:, :], imp3[:, :]), [], [b_sel])
                TS("dve", m8b[:, 7:8], m8b[:, 7:8], -1.0e29, None, ALU.max, W=[b_sel])
                TS("dve", selb4[:, qt, 64:96], imp2[:, :], m8b[:, 7:8], NEGB, ALU.is_lt, ALU.mult, R=[b_sel], W=[b_selb4[qt]])
            for r in range(4):
                kt_list = []
                for o in (-1, -2, -3, -4, 0, 1, 2, 3):
                    kt = q0 + o
                    if o < 0:
                        m_ = o + 4
                        rng_ = (0, 128 * (m_ + 1), 128 * m_, 128 * (m_ + 1))
                    else:
                        rng_ = (128 * o, 512, 128 * o, 128 * (o + 1))
                    kt_list.append((kwaT[0:100, kt * 128:(kt + 1) * 128], [b_kwa], cb[:, o + 4, :], vsw[:, kt, 1, :], [b_vsw[kt]], 128) + rng_)
                ai = branch(r, 2, kt_list, c)
                finalize(ai, r, 2, False, False)
                nfin[0] += 1
            for qt in range(4):
                pv_ = PS[I_][:, :].bitcast(BF16)
                TR(pv_[:, 0:128], selb4[:, qt, :], C["ident_b"][:], R=[b_selb4[qt], b_const], W=[bPS[I_]])
                for r in range(4):
                    CP("act", qaT[r][64:96, c * 512 + qt * 128:c * 512 + (qt + 1) * 128], pv_[64:96, 0:128],
                       R=[bPS[I_]], PW=[b_qsel[r]])
            for r in range(4):
                kt_list = []
                for kt in range(0, q0 + 4):
                    o = kt - q0
                    rng_ = (128 * o, 512, 128 * o, 128 * (o + 1)) if o >= 0 else (0, 512, 0, 512)
                    kt_list.append((ksaT[0:100, kt * 128:(kt + 1) * 128], [b_ksa], cb[:, 4 + o, :] if o >= 0 else None,
                                    vsw[:, kt, 0, :], [b_vsw[kt]], 128) + rng_)
                ai = branch(r, 1, kt_list, c)
                finalize(ai, r, 1, False, nfin[0] == 11)
                nfin[0] += 1
            TS("dve", Dsb[64:76, :], dacc[64:76, :], 1e-30, None, ALU.max, R=[b_dacc], W=[b_Dsb])
            RECIP(Dsb[64:76, :], Dsb[64:76, :], R=[b_Dsb], W=[b_Dsb])
            TT("dve", fT[64:76, :], Dsb[64:76, :], gT[64:76, c * 512:(c + 1) * 512], ALU.mult, R=[b_Dsb, b_gT], W=[b_fT])
            for pair in range(2):
                for br in range(3):
                    k6 = pair * 3 + br
                    fb = [I_, D_][fb_i[0] % 2]
                    fb_i[0] += 1
                    MM(PS[fb][:, :], sel12p[64:76, k6 * 128:(k6 + 1) * 128], fT[64:76, :], R=[b_c2, b_fT], W=[bPS[fb]])
                    ti = tm_i[0] % 2
                    tm_i[0] += 1
                    TT("dve", tmul[ti][:, :], numb[:, k6, :], PS[fb][:, :], ALU.mult, R=[b_numb[k6], bPS[fb]], W=[b_tmul[ti]])
                    MM(PS[M_][:, :], C["ident_b"][:, :], tmul[ti][:, :], start=(br == 0), stop=(br == 2),
                       R=[b_const, b_tmul[ti]], W=[bPS[M_]] if br == 0 else (), PW=() if br == 0 else [bPS[M_]])
                mc = 2 * g + pair
                CP("act", mixT[:, mc, c * 512:(c + 1) * 512], PS[M_][:, :], R=[bPS[M_]], PW=[b_mix[mc]])
    print("SBUF KiB used end of NSA", (A.cur - A.lo) / 1024.0)
    if "mixnsa" in debug:
        o = dout("dbg_mixnsa", [128, 8, NOWN], BF16)
        bd = Buf("dbg_mixnsa")
        for kc in range(8):
            DMA("sp", o[:, kc, :], mixT[:, kc, :], R=b_mix[0:8], PW=[bd])
    P.barrier()
    A.release(m_big)
    if stage <= 3:
        P.finish()
        P.replay()
        return nc, OUTS

    w_out = din("w_out", [D, D])
    w_out_v = w_out.rearrange("(kc p) n -> p kc n", p=128)
    n2w_d = din("n2w_row", [1, D])
    nfw_d = din("nfw_row", [1, D])
    wr_d = din("wr", [D, 36])
    br_d = din("br", [1, 36])

    x1 = A.alloc("x1", [128, 8, D], F32)
    b_x1 = [Buf("x1_%d" % i) for i in range(8)]
    m_p3 = A.mark()
    for i in range(8):
        DMA("sp", x1[:, i, :], xctx[1024 + i * 128:1024 + (i + 1) * 128, :], W=[b_x1[i]])
    g1bc = A.alloc("g1bc", [128, D], F32)
    b_g1 = Buf("g1bc")
    dgt[0] = None
    ada_rebuild(0, g1bc, b_g1)
    ytmp = [A.alloc("ytmp%d" % i, [128, 512], F32) for i in range(2)]
    b_yt = [Buf("ytmp%d" % i) for i in range(2)]
    n3 = 0
    for j in range(4):
        slot, bslot = ring_next()
        sv = v16(slot)
        DMA("pool", sv, w_out_v[:, :, j * 512:(j + 1) * 512], W=[bslot])
        for i in range(8):
            psi = 2 + n3 % 2
            yi = n3 % 2
            n3 += 1
            for kc in range(16):
                MM(PS[psi][:, :], mixT[:, kc, i * 128:(i + 1) * 128], sv[:, kc, :], start=(kc == 0), stop=(kc == 15),
                   R=[bslot, b_mix[kc]], W=[bPS[psi]] if kc == 0 else (), PW=() if kc == 0 else [bPS[psi]])
            TT("dve", ytmp[yi][:, :], PS[psi][:, :], g1bc[:, j * 512:(j + 1) * 512], ALU.mult, R=[bPS[psi], b_g1], W=[b_yt[yi]])
            TT("dve", x1[:, i, j * 512:(j + 1) * 512], ytmp[yi][:, :], x1[:, i, j * 512:(j + 1) * 512], ALU.add,
               R=[b_yt[yi], b_x1[i]], W=[b_x1[i]])
    if "x1" in debug:
        o = dout("dbg_x1", [128, 8, D], F32)
        bd = Buf("dbg_x1")
        for i in range(8):
            DMA("sp", o[:, i, :], x1[:, i, :], R=[b_x1[i]], PW=[bd])
    P.barrier()
    A.release(m_p3)
    if stage <= 4:
        P.finish()
        P.replay()
        return nc, OUTS

    h2b = A.alloc_at("h2b", [128, 8, D], BF16, off_mix)
    b_h2b = [Buf("h2b%d" % i) for i in range(8)]
    g2bc = A.alloc("g2bc", [128, D], F32)
    b_g2 = Buf("g2bc")
    Wc = A.alloc("Wc", [128, 8, 32], F32)
    posm = A.alloc("posm", [128, 8, 32], F32)
    selm = A.alloc("selm", [128, 8, 32], F32)
    selmb = A.alloc("selmb", [128, 8, 32], BF16)
    b_rt = [Buf("rt%d" % i) for i in range(8)]
    b_selmb = Buf("selmb")
    b_posm = [Buf("posm%d" % i) for i in range(8)]
    st2 = A.alloc("st2", [128, 8, 4], F32)
    b_st2 = [Buf("st2_%d" % i) for i in range(8)]
    m_p4 = A.mark()
    A2bc = A.alloc("A2bc", [128, D], F32)
    sh2bc = A.alloc("sh2bc", [128, D], F32)
    n2wbc = A.alloc("n2wbc", [128, D], F32)
    b_A2, b_sh2, b_n2w = Buf("A2"), Buf("sh2"), Buf("n2w")
    DMA("sp", n2wbc[:, :], n2w_d.partition_broadcast(128), W=[b_n2w])
    dgt[0] = None
    ada_rebuild(1, sh2bc, b_sh2)
    ada_rebuild(2, A2bc, b_A2)
    ada_rebuild(3, g2bc, b_g2)
    STT(A2bc[:, :], A2bc[:, :], 1.0, n2wbc[:, :], ALU.add, ALU.mult, R=[b_A2, b_n2w], W=[b_A2])
    h2f0 = A.alloc("h2f", [128, D], F32)
    h2fs = [h2f0, n2wbc]
    b_h2fs = [Buf("h2f"), b_n2w]
    h2T = A.alloc("h2T", [128, 16, 128], F32)
    b_h2T = Buf("h2T")
    wr = A.alloc("wr", [128, 16, 36], F32)
    brt = A.alloc("brt", [1, 36], F32)
    b_wr = Buf("wr")
    P.dma("sp", [("sp", lambda e: e.dma_start(out=wr[:, :, :], in_=wr_d.rearrange("(kc p) n -> p kc n", p=128))),
                 ("sp", lambda e: e.dma_start(out=brt[0:1, :], in_=br_d))], writes=[b_wr])
    rsets = []
    for k_ in range(2):
        rsets.append((A.alloc("lg", [128, 36], F32), A.alloc("rs", [128, 16], F32), A.alloc("gm", [128, 4], F32),
                      A.alloc("msk", [128, 32], F32), A.alloc("m8r", [128, 8], F32), A.alloc("s1w", [128, 32], F32),
                      A.alloc("s2w", [128, 32], F32), Buf("routescratch%d" % k_)))
    def p4_norm(i):
        h2f, b_h2f = h2fs[i % 2], b_h2fs[i % 2]
        ACT(h2b[:, i, :], x1[:, i, :], AF.Square, R=[b_x1[i]], W=[b_h2b[i], b_st2[i]], accum=st2[:, i, 0:1])
        ACT(st2[:, i, 1:2], st2[:, i, 0:1], AF.Sqrt, R=[b_st2[i]], PW=[b_st2[i]], scale=1.0 / D, bias=EPS)
        RECIP(st2[:, i, 2:3], st2[:, i, 1:2], R=[b_st2[i]], PW=[b_st2[i]])
        STT(h2f[:, :], x1[:, i, :], st2[:, i, 2:3], A2bc[:, :], ALU.mult, ALU.mult, R=[b_x1[i], b_st2[i], b_A2], W=[b_h2f])
        TT("dve", h2f[:, :], h2f[:, :], sh2bc[:, :], ALU.add, R=[b_h2f, b_sh2], W=[b_h2f])
        CP("act", h2b[:, i, :], h2f[:, :], R=[b_h2f], W=[b_h2b[i]])

    def p4_pe(i):
        h2f, b_h2f = h2fs[i % 2], b_h2fs[i % 2]
        for kg in range(4):
            psi = kg % 2
            for k4 in range(4):
                kc = 4 * kg + k4
                TR(PS[psi][:, k4 * 128:(k4 + 1) * 128], h2f[:, kc * 128:(kc + 1) * 128], C["ident_f"][:],
                   R=[b_h2f, b_const], W=[bPS[psi]] if k4 == 0 else (), PW=() if k4 == 0 else [bPS[psi]])
            CP("act" if kg % 2 else "dve", h2T[:, 4 * kg:4 * kg + 4, :], PS[psi][:, :].rearrange("p (a b) -> p a b", a=4),
               R=[bPS[psi]], W=[b_h2T] if kg == 0 else (), PW=() if kg == 0 else [b_h2T])
        for kc in range(16):
            MM(PS[2][:, 0:36], h2T[:, kc, :], wr[:, kc, :], start=(kc == 0), stop=False, R=[b_h2T, b_wr],
               W=[bPS[2]] if kc == 0 else (), PW=() if kc == 0 else [bPS[2]])
        MM(PS[2][:, 0:36], C["ones_f"][0:1, :], brt[0:1, :], start=False, stop=True, R=[b_const, b_wr], PW=[bPS[2]])

    def p4_route(i):
        lg, rs, gm, msk, m8, s1w, s2w, b_r = rsets[i % 2]
        CP("dve", lg[:, :], PS[2][:, 0:36], R=[bPS[2]], W=[b_r])
        P.op("dve", lambda e, rs=rs, lg=lg: e.tensor_reduce(rs[:, 0:1], lg[:, 0:4], AX.X, ALU.max), [], [b_r])
        TS("dve", rs[:, 1:2], rs[:, 0:1], -1.0, None, ALU.mult, W=[b_r])
        ACT(gm[:, :], lg[:, 0:4], AF.Exp, R=[b_r], W=[b_r], bias=rs[:, 1:2], accum=rs[:, 2:3])
        RECIP(rs[:, 3:4], rs[:, 2:3], W=[b_r])
        TS("dve", gm[:, :], lg[:, 0:4], rs[:, 0:1], None, ALU.is_equal, W=[b_r])
        TS("dve", gm[:, :], gm[:, :], -1.0, 1.0e30, ALU.add, ALU.mult, W=[b_r])
        TT("dve", msk[:, :].rearrange("p (a b) -> p a b", a=4), lg[:, 4:36].rearrange("p (a b) -> p a b", a=4),
           gm[:, :].unsqueeze(2).broadcast_to([128, 4, 8]), ALU.add, W=[b_r])
        P.op("dve", lambda e, m8=m8, msk=msk: e.max(m8[:, :], msk[:, :]), [], [b_r])
        TT("dve", rs[:, 4:5], m8[:, 1:2], m8[:, 0:1], ALU.subtract, W=[b_r])
        ACT(rs[:, 5:6], rs[:, 4:5], AF.Exp, R=[b_r], W=[b_r])
        TS("dve", rs[:, 6:7], rs[:, 5:6], 1.0, None, ALU.add, W=[b_r])
        RECIP(rs[:, 7:8], rs[:, 6:7], W=[b_r])
        TT("dve", rs[:, 8:9], rs[:, 7:8], rs[:, 3:4], ALU.mult, W=[b_r])
        TT("dve", rs[:, 9:10], rs[:, 5:6], rs[:, 8:9], ALU.mult, W=[b_r])
        TS("dve", s1w[:, :], msk[:, :], m8[:, 0:1], rs[:, 8:9], ALU.is_equal, ALU.mult, W=[b_r])
        TS("dve", s2w[:, :], msk[:, :], m8[:, 1:2], rs[:, 9:10], ALU.is_equal, ALU.mult, W=[b_r])
        TT("dve", Wc[:, i, :], s1w[:, :], s2w[:, :], ALU.add, R=[b_r], W=[b_rt[i]])
        TS("dve", selm[:, i, :], msk[:, :], m8[:, 1:2], None, ALU.is_ge, R=[b_r], PW=[b_rt[i]])

    p4_norm(0)
    for i in range(8):
        p4_pe(i)
        if i + 1 < 8:
            p4_norm(i + 1)
        p4_route(i)
    CP("dve", selmb[:, :, :], selm[:, :, :], R=b_rt, W=[b_selmb])
    for i in range(8):
        sb0 = 4 * (i // 4)
        prev = list(range(sb0, i))
        MM(PS[3][:, 0:32], C["lstrict"][:, :], selmb[:, i, :], start=True, stop=(len(prev) == 0),
           R=[b_const, b_selmb], W=[bPS[3]])
        for n_, ip in enumerate(prev):
            MM(PS[3][:, 0:32], C["ones_b"][:, :], selmb[:, ip, :], start=False, stop=(n_ == len(prev) - 1),
               R=[b_const, b_selmb], PW=[bPS[3]])
        STT(posm[:, i, :], PS[3][:, 0:32], 1.0, selm[:, i, :], ALU.add, ALU.mult, R=[bPS[3], b_rt[i]], W=[b_posm[i]])
        TS("dve", posm[:, i, :], posm[:, i, :], -1.0, None, ALU.add, R=[b_posm[i]], W=[b_posm[i]])
    if "route" in debug:
        o = dout("dbg_Wc", [128, 8, 32], F32)
        DMA("sp", o, Wc[:, :, :], R=b_rt, W=[Buf("dbg_Wc")])
        o = dout("dbg_posm", [128, 8, 32], F32)
        DMA("sp", o, posm[:, :, :], R=b_posm, W=[Buf("dbg_posm")])
        o = dout("dbg_h2b", [128, 8, D], BF16)
        bd = Buf("dbg_h2b")
        for i in range(8):
            DMA("sp", o[:, i, :], h2b[:, i, :], R=[b_h2b[i]], PW=[bd])
    P.barrier()
    A.release(m_p4)
    if stage <= 5:
        P.finish()
        P.replay()
        return nc, OUTS

    weg = din("w_expert_gate", [32, D, 512])
    weu = din("w_expert_up", [32, D, 512])
    wed = din("w_expert_down", [32, 512, D])
    out_d = dout("out", [NOWN, D])
    ring.append(A.alloc("ring2", [128, 8192], BF16))
    bring.append(Buf("ring2"))
    NRING3 = 3

    def ring_next3():
        i = rstate[0] % NRING3
        rstate[0] += 1
        return ring[i], bring[i]

    PmS = [[A.alloc("Pm%d%d" % (p_, i), [128, 4, 128], BF16) for i in range(2)] for p_ in range(2)]
    PwS = [[A.alloc("Pw%d%d" % (p_, i), [128, 4, 128], BF16) for i in range(2)] for p_ in range(2)]
    b_PmS = [[Buf("Pm%d%d" % (p_, i)) for i in range(2)] for p_ in range(2)]
    b_PwS = [[Buf("Pw%d%d" % (p_, i)) for i in range(2)] for p_ in range(2)]

    def make_P(ex):
        Pm, Pw, b_Pm, b_Pw = PmS[ex % 2], PwS[ex % 2], b_PmS[ex % 2], b_PwS[ex % 2]
        for sb in range(2):
            for il in range(4):
                i = 4 * sb + il
                TS("dve", Pm[sb][:, il, :], C["iota"][:, :], posm[:, i, ex:ex + 1], None, ALU.is_equal,
                   R=[b_const, b_posm[i]], W=[b_Pm[sb]] if il == 0 else (), PW=() if il == 0 else [b_Pm[sb]])
                TS("dve", Pw[sb][:, il, :], C["iota"][:, :], posm[:, i, ex:ex + 1], Wc[:, i, ex:ex + 1], ALU.is_equal, ALU.mult,
                   R=[b_const, b_posm[i], b_rt[i]], W=[b_Pw[sb]] if il == 0 else (), PW=() if il == 0 else [b_Pw[sb]])

    make_P(0)
    xeT = [A.alloc("xeT%d" % i, [128, 16, 128], BF16) for i in range(2)]
    b_xe = [Buf("xeT%d" % i) for i in range(2)]
    PwT = A.alloc("PwT", [128, 2, 2, 512], BF16)
    b_PwT = [[Buf("PwT%d%d" % (a_, b_)) for b_ in range(2)] for a_ in range(2)]
    sg = [A.alloc("sg%d" % i, [128, 512], F32) for i in range(2)]
    b_sg = [Buf("sg%d" % i) for i in range(2)]
    actT = [A.alloc("actT%d" % i, [128, 4, 128], BF16) for i in range(2)]
    b_act = [Buf("actT%d" % i) for i in range(2)]
    ye = A.alloc("ye", [128, 2, 2, D], BF16)
    b_ye = [[Buf("ye%d%d" % (a_, b_)) for b_ in range(2)] for a_ in range(2)]
    (DA_, DB_, G0_, U0_, G1_, U1_, YY_, CC_) = range(8)
    GB = [(G0_, U0_), (G1_, U1_)]
    nd = [0]
    ny = [0]
    for e2 in range(16):
        for el in range(2):
            ex = 2 * e2 + el
            sg_, bg_ = ring_next3()
            wgv = v16(sg_)
            DMA("pool", wgv, weg[ex].rearrange("(kc p) n -> p kc n", p=128), W=[bg_])
            su_, bu_ = ring_next3()
            wuv = v16(su_)
            DMA("pool", wuv, weu[ex].rearrange("(kc p) n -> p kc n", p=128), W=[bu_])
            sd_, bd_ = ring_next3()
            wdv = sd_[:, :].rearrange("p (a b) -> p a b", a=4)
            DMA("pool", wdv, wed[ex].rearrange("(fc p) n -> p fc n", p=128), W=[bd_])
            Pm, Pw, b_Pm, b_Pw = PmS[ex % 2], PwS[ex % 2], b_PmS[ex % 2], b_PwS[ex % 2]
            for sb in range(2):
                for kg in range(4):
                    psi = [DA_, DB_][nd[0] % 2]
                    nd[0] += 1
                    for k4 in range(4):
                        kc = 4 * kg + k4
                        for il in range(4):
                            i = 4 * sb + il
                            MM(PS[psi][:, k4 * 128:(k4 + 1) * 128], h2b[:, i, kc * 128:(kc + 1) * 128], Pm[sb][:, il, :],
                               start=(il == 0), stop=(il == 3), R=[b_h2b[i], b_Pm[sb]],
                               W=[bPS[psi]] if (k4 == 0 and il == 0) else (), PW=() if (k4 == 0 and il == 0) else [bPS[psi]])
                    CP("act" if kg % 2 else "dve", xeT[sb][:, 4 * kg:4 * kg + 4, :], PS[psi][:, :].rearrange("p (a b) -> p a b", a=4),
                       R=[bPS[psi]], W=[b_xe[sb]] if kg == 0 else (), PW=() if kg == 0 else [b_xe[sb]])
            for sb in range(2):
                psi = [DA_, DB_][nd[0] % 2]
                nd[0] += 1
                pvT = PS[psi][:, :].bitcast(BF16)
                for il in range(4):
                    TR(pvT[:, il * 128:(il + 1) * 128], Pw[sb][:, il, :], C["ident_b"][:], R=[b_Pw[sb], b_const],
                       W=[bPS[psi]] if il == 0 else (), PW=() if il == 0 else [bPS[psi]])
                CP("act", PwT[:, sb, el, :], pvT[:, 0:512], R=[bPS[psi]], W=[b_PwT[sb][el]])
            if ex + 1 < 32:
                make_P(ex + 1)
            for sb in range(2):
                for (wv, bw, psi) in ((wgv, bg_, GB[sb][0]), (wuv, bu_, GB[sb][1])):
                    for fc in range(4):
                        for kc in range(16):
                            MM(PS[psi][:, fc * 128:(fc + 1) * 128], wv[:, kc, fc * 128:(fc + 1) * 128], xeT[sb][:, kc, :],
                               start=(kc == 0), stop=(kc == 15), R=[bw, b_xe[sb]],
                               W=[bPS[psi]] if (fc == 0 and kc == 0) else (), PW=() if (fc == 0 and kc == 0) else [bPS[psi]])
                ACT(sg[sb][:, :], PS[GB[sb][0]][:, :], AF.Silu, R=[bPS[GB[sb][0]]], W=[b_sg[sb]])
                TT("dve", actT[sb][:, :, :].rearrange("p a b -> p (a b)"), sg[sb][:, :], PS[GB[sb][1]][:, :], ALU.mult,
                   R=[b_sg[sb], bPS[GB[sb][1]]], W=[b_act[sb]])
            for sb in range(2):
                for nb in range(4):
                    psi = [YY_, CC_][ny[0] % 2]
                    ny[0] += 1
                    for fc in range(4):
                        MM(PS[psi][:, :], actT[sb][:, fc, :], wdv[:, fc, nb * 512:(nb + 1) * 512], start=(fc == 0), stop=(fc == 3),
                           R=[b_act[sb], bd_], W=[bPS[psi]] if fc == 0 else (), PW=() if fc == 0 else [bPS[psi]])
                    TT("dve", ye[:, sb, el, nb * 512:(nb + 1) * 512], PS[psi][:, :], g2bc[:, nb * 512:(nb + 1) * 512], ALU.mult,
                       R=[bPS[psi], b_g2], W=[b_ye[sb][el]] if nb == 0 else (), PW=() if nb == 0 else [b_ye[sb][el]])
        for sb in range(2):
            for il in range(4):
                i = 4 * sb + il
                for nb in range(4):
                    psi = [YY_, CC_][ny[0] % 2]
                    ny[0] += 1
                    for el in range(2):
                        MM(PS[psi][:, :], PwT[:, sb, el, il * 128:(il + 1) * 128], ye[:, sb, el, nb * 512:(nb + 1) * 512],
                           start=(el == 0), stop=(el == 1), R=[b_PwT[sb][el], b_ye[sb][el]],
                           W=[bPS[psi]] if el == 0 else (), PW=() if el == 0 else [bPS[psi]])
                    TT("dve", x1[:, i, nb * 512:(nb + 1) * 512], PS[psi][:, :], x1[:, i, nb * 512:(nb + 1) * 512], ALU.add,
                       R=[bPS[psi], b_x1[i]], W=[b_x1[i]])
    print("SBUF KiB used end of MoE", (A.cur - A.lo) / 1024.0)
    P.barrier()
    A.release(m_p4)

    nfbc = A.alloc("nfbc", [128, D], F32)
    b_nf = Buf("nfbc")
    DMA("sp", nfbc[:, :], nfw_d.partition_broadcast(128), W=[b_nf])
    ost = [A.alloc("ost%d" % i, [128, D], F32) for i in range(2)]
    b_ost = [Buf("ost%d" % i) for i in range(2)]
    b_out = Buf("out")
    for i in range(8):
        oi = i % 2
        ACT(ost[oi][:, :], x1[:, i, :], AF.Square, R=[b_x1[i]], W=[b_ost[oi], b_st2[i]], accum=st2[:, i, 0:1])
        ACT(st2[:, i, 1:2], st2[:, i, 0:1], AF.Sqrt, R=[b_st2[i]], PW=[b_st2[i]], scale=1.0 / D, bias=EPS)
        RECIP(st2[:, i, 2:3], st2[:, i, 1:2], R=[b_st2[i]], PW=[b_st2[i]])
        STT(ost[oi][:, :], x1[:, i, :], st2[:, i, 2:3], nfbc[:, :], ALU.mult, ALU.mult, R=[b_x1[i], b_st2[i], b_nf], W=[b_ost[oi]])
        DMA("sp", out_d[i * 128:(i + 1) * 128, :], ost[oi][:, :], R=[b_ost[oi]], PW=[b_out], owner=b_ost[oi])
    print('SBUF peak KiB', (A.peak - A.lo) / 1024.0, 'of', (A.hi - A.lo) / 1024.0)
    P.finish()
    P.replay()
    return nc, OUTS


def core_inputs(inputs, b, s, consts):
    x = np.asarray(inputs["x"], np.float32)
    m = {}
    xc = np.zeros((NCTX, D), np.float32)
    if s == 1:
        xc[:] = x[b]
    else:
        xc[1024:] = x[b, :1024]
    m["xctx"] = xc
    m["cT"] = np.ascontiguousarray(np.asarray(inputs["c"], np.float32)[b].reshape(16, 128).T)
    m["w_ada"] = np.asarray(inputs["w_ada"], np.float32)[0]
    m["b_ada"] = np.ascontiguousarray(np.asarray(inputs["b_ada"], np.float32)[0].reshape(24, 512))
    m["n1w"] = np.ascontiguousarray(np.asarray(inputs["norm1_w"], np.float32)[0].reshape(16, 128).T)
    m["w_in"] = np.asarray(inputs["w_in"], np.float32)[0]
    m["w2aug"] = np.ascontiguousarray(np.concatenate([np.asarray(inputs["gla_w_gate2"], np.float32)[0],
                                                      np.asarray(inputs["gla_b_gate"], np.float32)[0][None, :]], axis=0))
    m["gnw"] = np.ascontiguousarray(np.asarray(inputs["gla_norm_w"], np.float32)[0].reshape(2, 128).T)
    w1k = np.asarray(inputs["cmp_w1_k"], np.float32)[0].reshape(32, 64, 64).transpose(1, 0, 2)
    w1v = np.asarray(inputs["cmp_w1_v"], np.float32)[0].reshape(32, 64, 64).transpose(1, 0, 2)
    m["cw1"] = np.ascontiguousarray(np.concatenate([w1k, w1v], axis=0))
    m["cw2"] = np.ascontiguousarray(np.concatenate([np.asarray(inputs["cmp_w2_k"], np.float32)[0],
                                                    np.asarray(inputs["cmp_w2_v"], np.float32)[0]], axis=0))
    m["cposT"] = np.ascontiguousarray(np.concatenate([np.asarray(inputs["cmp_pos_k"], np.float32)[0].T,
                                                      np.asarray(inputs["cmp_pos_v"], np.float32)[0].T], axis=0))
    m["w_out"] = np.asarray(inputs["w_out"], np.float32)[0]
    m["n2w_row"] = np.asarray(inputs["norm2_w"], np.float32)[0].reshape(1, D)
    m["nfw_row"] = np.asarray(inputs["norm_f_w"], np.float32).reshape(1, D)
    m["wr"] = np.ascontiguousarray(np.concatenate([np.asarray(inputs["w_router_group"], np.float32)[0],
                                                   np.asarray(inputs["w_router_expert"], np.float32)[0]], axis=1))
    m["br"] = np.ascontiguousarray(np.concatenate([np.asarray(inputs["b_router_group"], np.float32)[0],
                                                   np.asarray(inputs["b_router_expert"], np.float32)[0]])[None, :])
    m["w_expert_gate"] = np.asarray(inputs["w_expert_gate"], np.float32)[0]
    m["w_expert_up"] = np.asarray(inputs["w_expert_up"], np.float32)[0]
    m["w_expert_down"] = np.asarray(inputs["w_expert_down"], np.float32)[0]
    for k, v in consts[s].items():
        m["c_" + k] = v
    return m


_CACHE = {}


def kernel(**inputs):
    if "nc" not in _CACHE:
        _CACHE["nc"] = build()[0]
        _CACHE["consts"] = [make_consts(0), make_consts(1)]
    nc = _CACHE["nc"]
    consts = _CACHE["consts"]
    maps = [core_inputs(inputs, c // 2, c % 2, consts) for c in range(8)]
    res = run_bass_kernel_spmd(nc, maps, core_ids=list(range(8)))
    out = np.zeros((4, SEQ, D), np.float32)
    for c in range(8):
        b, sh = c // 2, c % 2
        out[b, sh * 1024:(sh + 1) * 1024] = np.asarray(res.results[c]["out"], np.float32)
    return out
```

```python
import numpy as np
import ml_dtypes
import concourse.bass as bass
import concourse.mybir as mybir
from concourse.bass_utils import run_bass_kernel_spmd

F32 = mybir.dt.float32
BF16 = mybir.dt.bfloat16
U32 = mybir.dt.uint32
AF = mybir.ActivationFunctionType
ALU = mybir.AluOpType
AX = mybir.AxisListType
NPBF = ml_dtypes.bfloat16

D = 2048
SEQ = 2048
NCTX = 2048
NOWN = 1024
KC = 16
EPS = 1e-6
NEGB = -30000.0
IN_WIDTH = 5696
OFF_Q, OFF_KV, OFF_G = 0, 1024, 2560
OFF_GQ, OFF_GK, OFF_GV, OFF_GO, OFF_LR = 2608, 3120, 3632, 4656, 5680
MOE_NBLK = 1
SAME_ENGINE_SYNC = True


class Buf:
    __slots__ = ("name", "w", "r", "dsem", "excl")

    def __init__(self, name, excl=False):
        self.name = name
        self.excl = excl
        self.w = {}
        self.r = {}
        self.dsem = None


class Prog:
    ENGS = ("sp", "act", "pool", "dve", "pe")

    def __init__(self, nc):
        self.nc = nc
        self.ops = {e: [] for e in self.ENGS}
        self.sems = {}
        self.cnt = {}
        self.seen = {e: {} for e in self.ENGS}
        self.pending = {e: {} for e in self.ENGS}
        for e in self.ENGS:
            self._mksem("E_" + e)
        self.ndsem = 0

    def _mksem(self, key):
        self.sems[key] = self.nc.alloc_semaphore(key)
        self.cnt[key] = 0
        return key

    def _need(self, eng, waits, key, val):
        if key == "E_" + eng and (eng == "pe" or not SAME_ENGINE_SYNC):
            return
        if self.seen[eng].get(key, 0) >= val:
            return
        if waits.get(key, 0) < val:
            waits[key] = val

    def _collect(self, eng, reads, writes, pwrites, extra=None):
        waits = dict(self.pending[eng])
        self.pending[eng] = {}
        for b in reads:
            for k, v in b.w.items():
                self._need(eng, waits, k, v)
            if b.excl:
                for k, v in b.r.items():
                    if k != "E_" + eng:
                        self._need(eng, waits, k, v)
        for b in writes:
            for k, v in b.w.items():
                self._need(eng, waits, k, v)
            for k, v in b.r.items():
                self._need(eng, waits, k, v)
        for b in pwrites:
            for k, v in b.r.items():
                self._need(eng, waits, k, v)
        if extra is not None:
            self._need(eng, waits, extra[0], extra[1])
        for k, v in waits.items():
            if self.seen[eng].get(k, 0) < v:
                self.seen[eng][k] = v
        return waits

    def _commit(self, key, val, reads, writes, pwrites):
        for b in reads:
            b.r[key] = val
        for b in writes:
            b.w = {key: val}
            b.r = {}
        for b in pwrites:
            b.w[key] = val

    def op(self, eng, fn, reads=(), writes=(), pwrites=()):
        waits = self._collect(eng, reads, writes, pwrites)
        key = "E_" + eng
        self.cnt[key] += 1
        self._commit(key, self.cnt[key], reads, writes, pwrites)
        self.ops[eng].append((list(waits.items()), fn, (key, 1)))

    def dma(self, q, fns, reads=(), writes=(), pwrites=(), owner=None):
        if callable(fns):
            fns = [(q, fns)]
        if owner is None:
            owner = writes[0] if writes else (pwrites[0] if pwrites else reads[0])
        if owner.dsem is None:
            self.ndsem += 1
            owner.dsem = self._mksem("D%d" % self.ndsem)
        dkey = owner.dsem
        prev = self.cnt[dkey]
        for (qq, fn) in fns:
            waits = self._collect(qq, reads, writes, pwrites, extra=(dkey, prev) if prev else None)
            self.ops[qq].append((list(waits.items()), fn, (dkey, 16)))
        self.cnt[dkey] += 16 * len(fns)
        self._commit(dkey, self.cnt[dkey], reads, writes, pwrites)

    def barrier(self):
        for e in self.ENGS:
            for k, v in self.cnt.items():
                if not v:
                    continue
                if k == "E_" + e and (e == "pe" or not SAME_ENGINE_SYNC):
                    continue
                if self.seen[e].get(k, 0) < v and self.pending[e].get(k, 0) < v:
                    self.pending[e][k] = v

    def finish(self):
        self.barrier()
        for e in self.ENGS:
            waits = self.pending[e]
            self.pending[e] = {}
            for k, v in waits.items():
                self.seen[e][k] = v
            if waits:
                self.ops[e].append((list(waits.items()), None, None))

    def replay(self):
        nc = self.nc
        P = self

        def run(ename, eng):
            for waits, fn, inc in P.ops[ename]:
                for k, v in waits:
                    eng.wait_ge(P.sems[k], v)
                if fn is None:
                    continue
                ins = fn(eng)
                if inc is not None:
                    ins.then_inc(P.sems[inc[0]], inc[1])

        with nc.Block() as block:
            @block.sync
            def _(e):
                run("sp", e)

            @block.scalar
            def _(e):
                run("act", e)

            @block.gpsimd
            def _(e):
                run("pool", e)

            @block.vector
            def _(e):
                run("dve", e)

            @block.tensor
            def _(e):
                run("pe", e)


class Arena:
    def __init__(self, nc):
        self.nc = nc
        self.lo = (nc.sbuf_base + 63) // 64 * 64
        self.hi = nc.sbuf_top
        self.cur = self.lo
        self.n = 0
        self.peak = self.lo

    def alloc(self, name, shape, dtype):
        esz = 2 if dtype == BF16 else 4
        per = int(np.prod(shape[1:])) * esz
        off = (self.cur + 63) // 64 * 64
        assert off + per <= self.hi, "SBUF overflow %s need %d at %d (hi %d)" % (name, per, off, self.hi)
        self.n += 1
        t = self.nc.alloc_sbuf_tensor_at("%s_%d" % (name, self.n), list(shape), dtype, offset=off)
        self.cur = off + per
        self.peak = max(self.peak, self.cur)
        self.last_off = off
        return t

    def alloc_at(self, name, shape, dtype, off):
        self.n += 1
        return self.nc.alloc_sbuf_tensor_at("%s_%d" % (name, self.n), list(shape), dtype, offset=off)

    def mark(self):
        return self.cur

    def release(self, m):
        self.cur = m


def _slopes():
    return (2.0 ** (-8.0 * np.arange(1, 17) / 16)).astype(np.float32)


def make_consts(s):
    c = {}
    t = np.arange(NCTX)
    first_real = 0 if s == 1 else 1024
    ka = np.zeros((36, NCTX), np.float32)
    ka[np.arange(NCTX) // 64, np.arange(NCTX)] = 1.0
    ka[32] = 64.0 * (t // 64)
    ka[33] = t % 64
    ka[34] = np.where(t < first_real, NEGB, 0.0)
    ka[35] = 1.0
    c["kaug"] = ka.astype(NPBF)
    n = np.arange(128)
    kc = np.zeros((36, 128), np.float32)
    kc[32] = 16.0 * n
    kc[34] = np.where((16 * n < first_real) | (n >= 127), NEGB, 0.0)
    kc[35] = 1.0
    c["kcaug"] = kc.astype(NPBF)
    sl = _slopes()
    tq = (1024 + np.arange(NOWN)).astype(np.float32)
    qa = np.zeros((16, 4, NOWN), np.float32)
    for h in range(16):
        slb = np.float32(sl[h]).astype(NPBF).astype(np.float32)
        qa[h, 0] = slb
        qa[h, 1] = slb
        qa[h, 2] = 1.0
        qa[h, 3] = -slb * tq
    c["qaug"] = qa.astype(NPBF)
    k = np.arange(128)[:, None]
    q = np.arange(512)[None, :]
    cb = np.zeros((8, 128, 512), np.float32)
    for i, o in enumerate(range(-4, 0)):
        cb[i] = np.where(q - (128 * o + k) >= 512, NEGB, 0.0)
    for i, o in enumerate(range(0, 4)):
        cb[4 + i] = np.where((128 * o + k) > q, NEGB, 0.0)
    c["cb"] = cb.astype(NPBF)
    cbc = np.zeros((2, 128, 512), np.float32)
    for ch in range(2):
        tqq = 1024 + 512 * ch + q
        cbc[ch] = np.where(16 * k + 31 > tqq, NEGB, 0.0)
    c["cbc"] = cbc.astype(NPBF)
    Fm = np.zeros((128, 8, 32), np.float32)
    Um = np.zeros((128, 8, 32), np.float32)
    fb = first_real // 64
    for i in range(8):
        tt = 1024 + 128 * i + np.arange(128)
        qb = (tt // 64)[:, None]
        j = np.arange(32)[None, :]
        forced = ((j == fb) | (j == qb) | (j == qb - 1)) & (j >= fb) & (j <= qb)
        Fm[:, i, :] = np.where(forced, 1e30, -3e38)
        Um[:, i, :] = np.where((j >= fb) & (j <= qb), 3e38, -1e30)
    c["selF"] = Fm
    c["selU"] = Um
    ov = np.zeros((128, 33), np.float32)
    for nn in range(127):
        for jj in range(32):
            if (16 * nn < 64 * jj + 64) and (16 * nn + 32 > 64 * jj):
                ov[nn, jj] = 1.0
        ov[nn, 32] = 1.0
    c["ovaug"] = ov.astype(NPBF)
    jj = np.arange(128)[:, None]
    ii = np.arange(128)[None, :]
    same = (jj // 64) == (ii // 64)
    c["glamask"] = (same & (jj <= ii)).astype(np.float32)
    L = np.zeros((128, 130), np.float32)
    L[:, :128] = np.where(same & (jj <= ii), -1.0 / 16, 0.0)
    L[:64, 128] = -1.0 / 16
    L[64:, 129] = -1.0 / 16
    c["lmat"] = L
    c["umat"] = np.where(same & (jj > ii), -1.0 / 16, 0.0).astype(np.float32)
    c["ident_f"] = np.eye(128, dtype=np.float32)
    c["ident_b"] = np.eye(128, dtype=np.float32).astype(NPBF)
    e64 = np.zeros((64, 192), np.float32)
    e64[np.arange(64), 64 + np.arange(64)] = 1.0
    c["e64"] = e64.astype(NPBF)
    c["oh12"] = np.eye(12, dtype=np.float32).reshape(1, 144)
    s12 = np.zeros((12, 12, 64), np.float32)
    for i in range(12):
        s12[i, i, :] = 1.0
    c["sel12"] = s12.astype(NPBF)
    sp_ = np.zeros((12, 6, 128), np.float32)
    for pair in range(2):
        for br in range(3):
            sp_[(2 * pair) * 3 + br, pair * 3 + br, 0:64] = 1.0
            sp_[(2 * pair + 1) * 3 + br, pair * 3 + br, 64:128] = 1.0
    c["selpair"] = sp_.astype(NPBF)
    c["ones_b"] = np.ones((128, 128), np.float32).astype(NPBF)
    c["ones_f"] = np.ones((128, 128), np.float32)
    c["iota"] = np.tile(np.arange(128, dtype=np.float32)[None, :], (128, 1))
    c["lstrict"] = (jj < ii).astype(np.float32).astype(NPBF)
    c["pvalid"] = np.full((128, 1), 1.0 if s == 1 else 0.0, np.float32)
    return c


CONST_DT = {"kaug": BF16, "kcaug": BF16, "qaug": BF16, "cb": BF16, "cbc": BF16, "ovaug": BF16,
            "ident_b": BF16, "e64": BF16, "sel12": BF16, "selpair": BF16, "ones_b": BF16, "lstrict": BF16}


def build(stage=99, debug=()):
    nc = bass.Bass("TRN2", target_bir_lowering=False)
    P = Prog(nc)
    A = Arena(nc)
    DR = {}
    OUTS = {}

    def din(name, shape, dt=F32):
        DR[name] = nc.dram_tensor(name, list(shape), dt, kind="ExternalInput").ap()
        return DR[name]

    def dout(name, shape, dt=F32):
        OUTS[name] = nc.dram_tensor(name, list(shape), dt, kind="ExternalOutput").ap()
        return OUTS[name]

    def MM(out, lhsT, rhs, start=True, stop=True, R=(), W=(), PW=()):
        P.op("pe", lambda e: e.matmul(out, lhsT, rhs, start=start, stop=stop), R, W, PW)

    def TR(out, in_, ident, R=(), W=(), PW=()):
        P.op("pe", lambda e: e.transpose(out, in_, ident), R, W, PW)

    def ACT(out, in_, func, R=(), W=(), PW=(), scale=None, bias=None, accum=None):
        kw = {}
        if scale is not None:
            kw["scale"] = scale
        if bias is not None:
            kw["bias"] = bias
        if accum is not None:
            kw["accum_out"] = accum
        P.op("act", lambda e: e.activation(out, in_, func, **kw), R, W, PW)

    def TT(eng, out, a, b, op, R=(), W=(), PW=()):
        P.op(eng, lambda e: e.tensor_tensor(out, a, b, op), R, W, PW)

    def TS(eng, out, a, s1, s2, op0, op1=None, R=(), W=(), PW=()):
        if op1 is None:
            P.op(eng, lambda e: e.tensor_scalar(out, a, s1, s2, op0), R, W, PW)
        else:
            P.op(eng, lambda e: e.tensor_scalar(out, a, s1, s2, op0, op1), R, W, PW)

    def STT(out, a, s, b, op0, op1, R=(), W=(), PW=()):
        P.op("dve", lambda e: e.scalar_tensor_tensor(out, a, s, b, op0, op1), R, W, PW)

    def CP(eng, out, in_, R=(), W=(), PW=()):
        if eng == "act":
            P.op("act", lambda e: e.copy(out, in_), R, W, PW)
        else:
            P.op(eng, lambda e: e.tensor_copy(out, in_), R, W, PW)

    def MS(eng, ap, val, W=(), PW=()):
        P.op(eng, lambda e: e.memset(ap, val), (), W, PW)

    def RECIP(out, in_, R=(), W=(), PW=()):
        P.op("dve", lambda e: e.reciprocal(out, in_), R, W, PW)

    def DMA(q, out, in_, R=(), W=(), PW=(), owner=None, **kw):
        P.dma(q, lambda e: e.dma_start(out=out, in_=in_, **kw), R, W, PW, owner)

    dbg_n = [0]

    def DBG(name, ap, shape, R, dt=F32):
        if name not in debug:
            return
        o = dout("dbg_" + name, shape, dt)
        DMA("sp", o, ap, R=R, W=[Buf("dbg_" + name)])

    PS = [nc.alloc_psum_tensor("psb%d" % i, [128, 512], F32) for i in range(8)]
    bPS = [Buf("ps%d" % i, excl=True) for i in range(8)]

    xctx = din("xctx", [NCTX, D])
    cT_d = din("cT", [128, 16])
    w_ada = din("w_ada", [D, 6 * D])
    b_ada = din("b_ada", [24, 512])
    n1w_d = din("n1w", [128, 16])
    w_ada_v = w_ada.rearrange("(kc p) n -> p kc n", p=128)
    consts_np = make_consts(0)
    CD = {}
    for k, v in consts_np.items():
        CD[k] = din("c_" + k, list(v.shape), CONST_DT.get(k, F32))

    b_const = Buf("consts")
    C = {}
    cfn = []

    def cload(name, shape, src_ap, dt=None):
        dt = dt or CONST_DT.get(name, F32)
        t = A.alloc("c_" + name, shape, dt)
        C[name] = t
        cfn.append(("sp", lambda e, t=t, s=src_ap: e.dma_start(out=t[:], in_=s)))
        return t

    cload("ident_f", [128, 128], CD["ident_f"])
    cload("ident_b", [128, 128], CD["ident_b"])
    cload("ones_b", [128, 128], CD["ones_b"])
    cload("ones_f", [128, 128], CD["ones_f"])
    cload("cT", [128, 16], cT_d, F32)
    cload("n1w", [128, 16], n1w_d, F32)
    w_in = din("w_in", [D, IN_WIDTH])
    w_in_v = w_in.rearrange("(kc p) n -> p kc n", p=128)
    w2aug_d = din("w2aug", [17, 512])
    gnw_d = din("gnw", [128, 2])
    cload("gnw", [128, 2], gnw_d, F32)
    cload("glamask", [128, 128], CD["glamask"])
    cload("lmat", [128, 130], CD["lmat"])
    cload("umat", [128, 128], CD["umat"])
    cload("e64", [64, 192], CD["e64"])
    cload("ovaug", [128, 33], CD["ovaug"])
    cload("iota", [128, 128], CD["iota"])
    cload("lstrict", [128, 128], CD["lstrict"])
    cload("pvalid", [128, 1], CD["pvalid"])
    P.dma("sp", cfn, writes=[b_const])

    NRING = 2
    ring = [A.alloc("ring%d" % i, [128, 8192], BF16) for i in range(NRING)]
    bring = [Buf("ring%d" % i) for i in range(NRING)]
    rstate = [0]

    def ring_next():
        i = rstate[0] % NRING
        rstate[0] += 1
        return ring[i], bring[i]

    def v16(t, n=512):
        return t[:, 0:16 * n].rearrange("p (a b) -> p a b", a=16)

    cols1 = A.alloc("cols1", [128, 2, 16], F32)
    b_cols1 = Buf("cols1")
    scl = A.alloc("scl", [128, 16], F32)
    b_scl = Buf("scl")
    adacols = A.alloc("adacols", [128, 64], F32)
    b_adac = Buf("adacols")
    brow = [A.alloc("brow%d" % i, [1, 512], BF16) for i in range(2)]
    b_brow = [Buf("brow%d" % i) for i in range(2)]
    mixT = A.alloc("mixT", [128, KC, NOWN], BF16)
    off_mix = A.last_off
    b_mix = [Buf("mix%d" % i) for i in range(16)]
    off_mixT = A.lo
    m_big = A.mark()
    hT = A.alloc("hT", [128, KC, NCTX], BF16)
    b_hT = [Buf("hT%d" % t) for t in range(16)]
    m_after_hT = A.mark()

    ACT(scl[:], C["cT"][:], AF.Silu, R=[b_const], W=[b_scl])
    screp_state = {}

    def make_screp():
        t = A.alloc("sc_rep", [128, 16, 128], BF16)
        b = Buf("screp")
        for kc in range(16):
            TS("dve", t[:, kc, :], C["ones_b"][:], scl[:, kc:kc + 1], None, ALU.mult,
               R=[b_const, b_scl], PW=[b])
        screp_state["t"] = t
        screp_state["b"] = b

    make_screp()
    m_tmp = A.mark()
    ada_n = [0]

    def ada_block(j, dst, dstbuf, psi, excl_dst=False):
        slot, bslot = ring_next()
        sv = v16(slot)
        DMA("pool", sv, w_ada_v[:, :, j * 512:(j + 1) * 512], W=[bslot])
        bi = ada_n[0] % 2
        ada_n[0] += 1
        DMA("pool", brow[bi][0:1, :], b_ada[j:j + 1, :], W=[b_brow[bi]])
        for kc in range(16):
            MM(PS[psi][:, :], screp_state["t"][:, kc, :], sv[:, kc, :], start=(kc == 0), stop=False,
               R=[bslot, screp_state["b"]], W=[bPS[psi]] if kc == 0 else (), PW=() if kc == 0 else [bPS[psi]])
        MM(PS[psi][:, :], C["ones_b"][0:1, :], brow[bi][0:1, :], start=False, stop=True,
           R=[b_const, b_brow[bi]], PW=[bPS[psi]])
        CP("act", dst, PS[psi][:, :], R=[bPS[psi]], PW=[dstbuf])

    dgt = [None]

    def ada_rebuild(v, dst, dstbuf):
        if dgt[0] is None:
            dgt[0] = ([A.alloc("dg%d" % i, [128, 128], F32) for i in range(2)], [Buf("dg%d" % i) for i in range(2)])
        dg, b_dg = dgt[0]
        for q4 in range(4):
            psi = q4 % 2
            for c4 in range(4):
                cc = 4 * q4 + c4
                di = cc % 2
                TS("dve", dg[di][:, :], C["ident_f"][:, :], adacols[:, 16 * v + cc:16 * v + cc + 1], None, ALU.mult,
                   R=[b_const, b_adac], W=[b_dg[di]])
                MM(PS[psi][:, c4 * 128:(c4 + 1) * 128], C["ones_f"][:, :], dg[di][:, :], R=[b_const, b_dg[di]],
                   W=[bPS[psi]] if c4 == 0 else (), PW=() if c4 == 0 else [bPS[psi]])
            CP("act", dst[:, q4 * 512:(q4 + 1) * 512], PS[psi][:, :], R=[bPS[psi]], PW=[dstbuf])

    bc1 = A.alloc("bc1", [128, 2, 2048], F32)
    b_bc1 = Buf("bc1")
    for j in range(8):
        ada_block(j, bc1[:, j // 4, (j % 4) * 512:(j % 4 + 1) * 512], b_bc1, j % 2)
    DBG("bc1", bc1[0:1, :, :], [1, 2, 2048], [b_bc1])
    dtmp = A.alloc("dtmp", [128, 16, 128], F32)
    b_dtmp = Buf("dtmp")
    col16 = A.alloc("col16", [128, 2, 16], F32)
    b_col16 = Buf("col16")
    idb = C["ident_f"][:, :].unsqueeze(1).broadcast_to([128, 16, 128])
    for which in range(2):
        TT("dve", dtmp[:, :, :], bc1[:, which, :].rearrange("p (a b) -> p a b", a=16), idb, ALU.mult,
           R=[b_bc1, b_const], W=[b_dtmp])
        P.op("dve", lambda e, w=which: e.tensor_reduce(col16[:, w, :], dtmp[:, :, :], AX.X, ALU.add),
             [b_dtmp], (), [b_col16])
    CP("dve", cols1[:, 0, :], col16[:, 0, :], R=[b_col16], PW=[b_cols1])
    STT(cols1[:, 1, :], col16[:, 1, :], 1.0, C["n1w"][:], ALU.add, ALU.mult,
        R=[b_col16, b_const], PW=[b_cols1])
    DBG("cols1", cols1[:, :, :], [128, 2, 16], [b_cols1])
    P.barrier()
    A.release(m_tmp)
    if stage <= 0:
        P.finish()
        P.replay()
        return nc, OUTS

    xt = [A.alloc("xt%d" % i, [128, D], F32) for i in range(2)]
    b_xt = [Buf("xt%d" % i) for i in range(2)]
    xs = [A.alloc("xs%d" % i, [128, D], BF16) for i in range(2)]
    b_xs = [Buf("xs%d" % i) for i in range(2)]
    junk = A.alloc("junk", [128, D], BF16)
    b_junk = Buf("junk")
    st1 = A.alloc("st1", [128, 16, 4], F32)
    b_st1 = [Buf("st1_%d" % t) for t in range(16)]
    import os
    PH1_T = int(os.environ.get('PH1_T', '16'))
    PH1_M = int(os.environ.get('PH1_M', '9'))
    adat = [A.alloc("adat%d" % i, [128, 512], F32) for i in range(2)]
    b_adat = [Buf("adat%d" % i) for i in range(2)]
    dt4 = A.alloc("dt4", [128, 4, 128], F32)
    b_dt4 = Buf("dt4")
    idb4 = C["ident_f"][:, :].unsqueeze(1).broadcast_to([128, 4, 128])
    for t in range(PH1_T):
        i2 = t % 2
        ada_block(8 + t, adat[i2][:, :], b_adat[i2], 4 + i2, excl_dst=True)
        TT("dve", dt4[:, :, :], adat[i2][:, :].rearrange("p (a b) -> p a b", a=4), idb4, ALU.mult,
           R=[b_adat[i2], b_const], W=[b_dt4])
        P.op("dve", lambda e, t=t: e.tensor_reduce(adacols[:, 4 * t:4 * t + 4], dt4[:, :, :], AX.X, ALU.add),
             [b_dt4], (), [b_adac])
        DMA("sp", xt[i2][:], xctx[t * 128:(t + 1) * 128, :], W=[b_xt[i2]])
        ACT(junk[:], xt[i2][:], AF.Square, R=[b_xt[i2]], W=[b_junk, b_st1[t]], accum=st1[:, t, 0:1])
        if PH1_M < 1:
            continue
        ACT(st1[:, t, 1:2], st1[:, t, 0:1], AF.Sqrt, R=[b_st1[t]], PW=[b_st1[t]], scale=1.0 / D, bias=EPS)
        if PH1_M < 2:
            continue
        RECIP(st1[:, t, 2:3], st1[:, t, 1:2], R=[b_st1[t]], PW=[b_st1[t]])
        TS("dve", xs[i2][:], xt[i2][:], st1[:, t, 2:3], None, ALU.mult, R=[b_xt[i2], b_st1[t]], W=[b_xs[i2]])
        if PH1_M < 3:
            continue
        for half in range(2):
            psi = 2 + half
            pv = PS[psi][:, :].bitcast(BF16)
            for k8 in range(8):
                kc = half * 8 + k8
                TR(pv[:, k8 * 128:(k8 + 1) * 128], xs[i2][:, kc * 128:(kc + 1) * 128], C["ident_b"][:],
                   R=[b_xs[i2], b_const], W=[bPS[psi]] if k8 == 0 else (), PW=() if k8 == 0 else [bPS[psi]])
            if PH1_M < 4:
                continue
            for k8 in range(8):
                kc = half * 8 + k8
                dst = hT[:, kc, t * 128:(t + 1) * 128]
                src = pv[:, k8 * 128:(k8 + 1) * 128]
                if half == 0:
                    ACT(dst, src, AF.Identity, R=[bPS[psi], b_cols1], PW=[b_hT[t]],
                        scale=cols1[:, 1, kc:kc + 1], bias=cols1[:, 0, kc:kc + 1])
                else:
                    TS("dve", dst, src, cols1[:, 1, kc:kc + 1], cols1[:, 0, kc:kc + 1], ALU.mult, ALU.add,
                       R=[bPS[psi], b_cols1], PW=[b_hT[t]])
    if "hT" in debug:
        o = dout("dbg_hT", [128, KC, NCTX], BF16)
        bd = Buf("dbg_hT")
        for kc in range(KC):
            DMA("sp", o[:, kc, :], hT[:, kc, :], R=b_hT, PW=[bd])
    P.barrier()
    m_tmp = m_after_hT
    A.release(m_tmp)
    if stage <= 1:
        P.finish()
        P.replay()
        return nc, OUTS

    DK = 128
    lrT = A.alloc("lrT", [32, NCTX], BF16)
    b_lrT = Buf("lrT")
    w2a = A.alloc("w2a", [32, 512], BF16)
    b_w2a = Buf("w2a")
    l_tok = A.alloc("l_tok", [128, 16, 128], F32)
    b_ltok = [Buf("ltok%d" % t) for t in range(16)]
    etmp = A.alloc("etmp", [128, 128], F32)
    b_etmp = Buf("etmp")
    ebT = A.alloc("ebT", [128, 512], F32)
    enbT = A.alloc("enbT", [128, 512], F32)
    b_eb = [Buf("eb%d" % i) for i in range(4)]
    ehat = A.alloc("ehat", [128, 4, 128], F32)
    b_ehat = [Buf("ehat%d" % i) for i in range(4)]
    dec = A.alloc("dec", [128, 16, 2], F32)
    b_dec = [Buf("dec%d" % t) for t in range(16)]
    khat = [A.alloc("khat%d" % i, [128, 4, 128], BF16) for i in range(2)]
    vtok = [A.alloc("vtok%d" % i, [128, 4, 256], BF16) for i in range(2)]
    b_kv = [[Buf("kv%d_%d" % (i, j)) for j in range(4)] for i in range(2)]
    qtT = A.alloc("qtT", [128, NOWN], F32)
    ktT = A.alloc("ktT", [128, NOWN], F32)
    b_qk = [Buf("qk%d" % i) for i in range(2)]
    ogT = A.alloc("ogT", [128, 2, NOWN], BF16)
    b_og = [Buf("og%d" % i) for i in range(2)]
    Sst = [A.alloc("S%d" % i, [128, 256], F32) for i in range(3)]
    b_S = [Buf("S%d" % i) for i in range(3)]
    ATm = A.alloc("ATm", [128, 128], BF16)
    b_ATm = Buf("ATm")
    oT_sb = A.alloc("oT_sb", [128, 2, 512], F32)
    b_oT = [Buf("oT%d" % i) for i in range(4)]
    sqb = A.alloc("sqb", [128, 2, 512], BF16)
    b_sq = Buf("sq")
    rsd = A.alloc("rsd", [128, 512], F32)
    b_rsd = Buf("rsd")
    t1b = A.alloc("t1b", [128, 512], F32)
    b_t1 = Buf("t1")

    MS("pool", lrT[:, :], 1.0, W=[b_lrT])
    DMA("pool", w2a[0:17, :], w2aug_d, W=[b_w2a])
    (Z_, G_, KV_, PA_, PB_, U_, A_, O2_) = range(8)
    projb = [PA_, PB_]
    pj = [0]

    def proj_fm(wv, c0, ncols, tok0, ntok, bslot, tiles):
        psi = projb[pj[0] % 2]
        pj[0] += 1
        for kc in range(16):
            MM(PS[psi][0:ncols, 0:ntok], wv[:, kc, c0:c0 + ncols], hT[:, kc, tok0:tok0 + ntok],
               start=(kc == 0), stop=(kc == 15), R=[bslot] + [b_hT[t] for t in tiles],
               W=[bPS[psi]] if kc == 0 else (), PW=() if kc == 0 else [bPS[psi]])
        return psi

    for hg in range(4):
        slotA, bA = ring_next()
        svA = v16(slotA)
        P.dma("pool", [("pool", lambda e, hg=hg: e.dma_start(out=svA[:, :, 0:128], in_=w_in_v[:, :, OFF_GQ + hg * 128:OFF_GQ + (hg + 1) * 128])),
                       ("pool", lambda e, hg=hg: e.dma_start(out=svA[:, :, 128:256], in_=w_in_v[:, :, OFF_GK + hg * 128:OFF_GK + (hg + 1) * 128])),
                       ("pool", lambda e, hg=hg: e.dma_start(out=svA[:, :, 256:512], in_=w_in_v[:, :, OFF_GV + hg * 256:OFF_GV + (hg + 1) * 256]))],
              writes=[bA])
        slotB, bB = ring_next()
        svB = v16(slotB)
        fnsB = [("pool", lambda e, hg=hg: e.dma_start(out=svB[:, :, 0:256], in_=w_in_v[:, :, OFF_GO + hg * 256:OFF_GO + (hg + 1) * 256]))]
        if hg == 0:
            fnsB.append(("pool", lambda e: e.dma_start(out=svB[:, :, 256:272], in_=w_in_v[:, :, OFF_LR:OFF_LR + 16])))
        P.dma("pool", fnsB, writes=[bB])
        if hg == 0:
            for cj in range(4):
                psi = proj_fm(svB, 256, 16, cj * 512, 512, bB, range(4 * cj, 4 * cj + 4))
                CP("act", lrT[0:16, cj * 512:(cj + 1) * 512], PS[psi][0:16, 0:512], R=[bPS[psi]], PW=[b_lrT])
        MS("dve", Sst[0][:, :], 0.0, W=[b_S[0]])
        scur = 0
        for cj in range(4):
            own = cj >= 2
            par = cj % 2
            oc0 = (cj - 2) * 512
            for tl in range(4):
                t = 4 * cj + tl
                MM(PS[Z_][:, 0:128], lrT[0:17, t * 128:(t + 1) * 128], w2a[0:17, hg * 128:(hg + 1) * 128],
                   R=[b_lrT, b_w2a], W=[bPS[Z_]])
                KVb = [KV_, PA_][t % 2]
                for kc in range(16):
                    MM(PS[KVb][:, 0:384], hT[:, kc, t * 128:(t + 1) * 128], svA[:, kc, 128:512],
                       start=(kc == 0), stop=(kc == 15), R=[bA, b_hT[t]],
                       W=[bPS[KVb]] if kc == 0 else (), PW=() if kc == 0 else [bPS[KVb]])
                ACT(etmp[:, :], PS[Z_][:, 0:128], AF.Exp, R=[bPS[Z_]], W=[b_etmp], scale=-1.0)
                ACT(l_tok[:, t, :], etmp[:, :], AF.Ln, R=[b_etmp], W=[b_ltok[t]], bias=1.0)
                MM(PS[G_][:, 0:130], l_tok[:, t, :], C["lmat"][:, 0:130], R=[b_ltok[t], b_const], W=[bPS[G_]])
                MM(PS[G_][:, 256:384], C["umat"][:, :], l_tok[:, t, :], R=[b_ltok[t], b_const], PW=[bPS[G_]])
                if own:
                    ACT(ebT[:, tl * 128:(tl + 1) * 128], PS[G_][:, 0:128], AF.Exp, R=[bPS[G_]], W=[b_eb[tl]])
                    ACT(enbT[:, tl * 128:(tl + 1) * 128], PS[G_][:, 0:128], AF.Exp, R=[bPS[G_]], PW=[b_eb[tl]], scale=-1.0)
                ACT(dec[:, t, :], PS[G_][:, 128:130], AF.Exp, R=[bPS[G_]], W=[b_dec[t]])
                ACT(ehat[:, tl, :], PS[G_][:, 256:384], AF.Exp, R=[bPS[G_]], W=[b_ehat[tl]])
                if own:
                    TT("dve", khat[par][:, tl, :], PS[KVb][:, 0:128], ehat[:, tl, :], ALU.mult,
                       R=[bPS[KVb], b_ehat[tl]], W=[b_kv[par][tl]])
                else:
                    STT(khat[par][:, tl, :], PS[KVb][:, 0:128], C["pvalid"][:, 0:1], ehat[:, tl, :], ALU.mult, ALU.mult,
                        R=[bPS[KVb], b_ehat[tl], b_const], W=[b_kv[par][tl]])
                CP("dve", vtok[par][:, tl, :], PS[KVb][:, 128:384], R=[bPS[KVb]], PW=[b_kv[par][tl]])
            if own:
                tiles = range(4 * cj, 4 * cj + 4)
                psi = proj_fm(svA, 0, 128, cj * 512, 512, bA, tiles)
                STT(qtT[:, oc0:oc0 + 512], PS[psi][:, 0:512], float(DK) ** -0.5, ebT[:, :], ALU.mult, ALU.mult,
                    R=[bPS[psi]] + b_eb, W=[b_qk[cj - 2]])
                psi = proj_fm(svA, 128, 128, cj * 512, 512, bA, tiles)
                TT("dve", ktT[:, oc0:oc0 + 512], PS[psi][:, 0:512], enbT[:, :], ALU.mult,
                   R=[bPS[psi]] + b_eb, PW=[b_qk[cj - 2]])
                for half in range(2):
                    psi = proj_fm(svB, half * 128, 128, cj * 512, 512, bB, tiles)
                    ACT(ogT[:, half, oc0:oc0 + 512], PS[psi][:, 0:512], AF.Silu, R=[bPS[psi]],
                        W=[b_og[cj - 2]] if half == 0 else (), PW=() if half == 0 else [b_og[cj - 2]])
            for tl in range(4):
                t = 4 * cj + tl
                MM(PS[U_][:, 0:256], khat[par][0:64, tl, :], vtok[par][0:64, tl, :], R=[b_kv[par][tl]], W=[bPS[U_]])
                MM(PS[A_][:, 256:512], khat[par][64:128, tl, :], vtok[par][64:128, tl, :], R=[b_kv[par][tl]], W=[bPS[A_]])
                s0, s1, s2 = scur, (scur + 1) % 3, (scur + 2) % 3
                if own:
                    tc0 = oc0 + tl * 128
                    MM(PS[A_][:, 0:128], ktT[:, tc0:tc0 + 128], qtT[:, tc0:tc0 + 128], R=[b_qk[cj - 2]], PW=[bPS[A_]])
                    TT("dve", ATm[:, :], PS[A_][:, 0:128], C["glamask"][:, :], ALU.mult, R=[bPS[A_], b_const], W=[b_ATm])
                STT(Sst[s1][:, :], Sst[s0][:, :], dec[:, t, 0:1], PS[U_][:, 0:256], ALU.mult, ALU.add,
                    R=[b_S[s0], b_dec[t], bPS[U_]], W=[b_S[s1]])
                STT(Sst[s2][:, :], Sst[s1][:, :], dec[:, t, 1:2], PS[A_][:, 256:512], ALU.mult, ALU.add,
                    R=[b_S[s1], b_dec[t], bPS[A_]], W=[b_S[s2]])
                if own:
                    for half in range(2):
                        hs = slice(half * 128, (half + 1) * 128)
                        MM(PS[O2_][:, hs], vtok[par][:, tl, hs], ATm[:, :], start=True, stop=False,
                           R=[b_kv[par][tl], b_ATm], W=[bPS[O2_]] if half == 0 else (), PW=() if half == 0 else [bPS[O2_]])
                        MM(PS[O2_][:, half * 128:half * 128 + 64], Sst[s0][:, hs], qtT[:, tc0:tc0 + 64],
                           start=False, stop=False, R=[b_S[s0], b_qk[cj - 2]], PW=[bPS[O2_]])
                        MM(PS[O2_][:, half * 128 + 64:half * 128 + 128], Sst[s1][:, hs], qtT[:, tc0 + 64:tc0 + 128],
                           start=False, stop=True, R=[b_S[s1], b_qk[cj - 2]], PW=[bPS[O2_]])
                    CP("act", oT_sb[:, :, tl * 128:(tl + 1) * 128],
                       PS[O2_][:, 0:256].rearrange("p (a b) -> p a b", a=2), R=[bPS[O2_]], W=[b_oT[tl]])
                scur = s2
            if own:
                ACT(sqb[:, :, :], oT_sb[:, :, :], AF.Square, R=b_oT, W=[b_sq])
                MM(PS[Z_][:, 0:512], C["ones_b"][:, :], sqb[:, 0, :], start=True, stop=False, R=[b_const, b_sq], W=[bPS[Z_]])
                MM(PS[Z_][:, 0:512], C["ones_b"][:, :], sqb[:, 1, :], start=False, stop=True, R=[b_const, b_sq], PW=[bPS[Z_]])
                ACT(rsd[:, :], PS[Z_][:, 0:512], AF.Sqrt, R=[bPS[Z_]], W=[b_rsd], scale=1.0 / 256, bias=EPS)
                RECIP(rsd[:, :], rsd[:, :], R=[b_rsd], W=[b_rsd])
                for half in range(2):
                    TT("dve", t1b[:, :], oT_sb[:, half, :], rsd[:, :], ALU.mult, R=b_oT + [b_rsd], W=[b_t1])
                    mc = 8 + 2 * hg + half
                    STT(mixT[:, mc, oc0:oc0 + 512], t1b[:, :], C["gnw"][:, half:half + 1], ogT[:, half, oc0:oc0 + 512],
                        ALU.mult, ALU.mult, R=[b_t1, b_const, b_og[cj - 2]], PW=[b_mix[mc]])
    print("SBUF KiB used end of GLA", (A.cur - A.lo) / 1024.0)
    if "mixgla" in debug:
        o = dout("dbg_mixgla", [128, 8, NOWN], BF16)
        bd = Buf("dbg_mixgla")
        for kc in range(8):
            DMA("sp", o[:, kc, :], mixT[:, 8 + kc, :], R=b_mix[8:16], PW=[bd])
    P.barrier()
    A.release(m_tmp)
    if stage <= 2:
        P.finish()
        P.replay()
        return nc, OUTS

    cw1_d = din("cw1", [128, 32, 64])
    cw2_d = din("cw2", [128, 64])
    cpos_d = din("cposT", [128, 32])
    b_c2 = Buf("consts2")
    cb = A.alloc("cb", [128, 8, 512], BF16)
    cbc = A.alloc("cbc", [128, 2, 512], BF16)
    selF = A.alloc("selF", [128, 8, 32], F32)
    selU = A.alloc("selU", [128, 8, 32], F32)
    ksaT = A.alloc("ksaT", [100, NCTX], BF16)
    kwaT = A.alloc("kwaT", [100, NCTX], BF16)
    kcaT = A.alloc("kcaT", [100, 128], BF16)
    b_ksa, b_kwa, b_kca = Buf("ksaT"), Buf("kwaT"), Buf("kcaT")
    vcaug = A.alloc("vcaug", [128, 76], BF16)
    b_vca = Buf("vcaug")
    qaT = [A.alloc("qaT%d" % r, [100, NOWN], BF16) for r in range(4)]
    b_qa = [Buf("qaT%d" % r) for r in range(4)]
    b_qsel = [Buf("qsel%d" % r) for r in range(4)]
    kvcT = A.alloc("kvcT", [128, NCTX], BF16)
    b_kvc = Buf("kvcT")
    vsw = A.alloc("vsw", [128, 16, 2, 76], BF16)
    b_vsw = [Buf("vsw%d" % t) for t in range(16)]
    gT = A.alloc("gT", [76, NOWN], BF16)
    b_gT = Buf("gT")
    cw1 = A.alloc("cw1", [128, 32, 64], BF16)
    cw2 = A.alloc("cw2", [128, 64], BF16)
    cposT = A.alloc("cposT", [128, 32], BF16)
    cbias = A.alloc("cbias", [128, 1], F32)
    b_cbias = Buf("cbias")
    gu = A.alloc("gu", [128, 128], F32)
    gq = A.alloc("gq", [128, 128], F32)
    gl = A.alloc("gl", [128, 128], BF16)
    b_gu, b_gq, b_gl = Buf("gu"), Buf("gq"), Buf("gl")
    NPT = 4
    pTb = [A.alloc("pT%d" % i, [128, 512], BF16) for i in range(NPT)]
    b_pT = [Buf("pT%d" % i) for i in range(NPT)]
    numb = A.alloc("numb", [128, 6, 512], BF16)
    b_numb = [Buf("numb%d" % i) for i in range(6)]
    dacc = A.alloc("dacc", [76, 512], F32)
    b_dacc = Buf("dacc")
    Dsb = A.alloc("Dsb", [76, 512], F32)
    b_Dsb = Buf("Dsb")
    fT = A.alloc("fT", [76, 512], BF16)
    b_fT = Buf("fT")
    ohcol = A.alloc("ohcol", [76, 12], F32)
    sel12p = A.alloc("sel12p", [76, 768], BF16)
    tmul = [A.alloc("tmul%d" % i, [128, 512], BF16) for i in range(2)]
    b_tmul = [Buf("tmul%d" % i) for i in range(2)]
    impt = A.alloc("impt", [128, 4, 32], F32)
    b_imp = [Buf("imp%d" % i) for i in range(4)]
    sm = A.alloc("sm", [128, 4, 8], F32)
    b_sm = [Buf("sm%d" % i) for i in range(4)]
    imp2 = A.alloc("imp2", [128, 32], F32)
    imp3 = A.alloc("imp3", [128, 32], F32)
    m8a = A.alloc("m8a", [128, 8], F32)
    m8b = A.alloc("m8b", [128, 8], F32)
    b_sel = Buf("selscratch")
    selb4 = A.alloc("selb4", [128, 4, 128], BF16)
    b_selb4 = [Buf("selb%d" % i) for i in range(4)]

    fns = [("sp", lambda e: e.dma_start(out=cb[:, :, :], in_=CD["cb"].rearrange("i k q -> k i q"))),
           ("sp", lambda e: e.dma_start(out=cbc[:, :, :], in_=CD["cbc"].rearrange("i k q -> k i q"))),
           ("sp", lambda e: e.dma_start(out=selF[:, :, :], in_=CD["selF"])),
           ("sp", lambda e: e.dma_start(out=selU[:, :, :], in_=CD["selU"])),
           ("sp", lambda e: e.dma_start(out=ksaT[64:100, :], in_=CD["kaug"])),
           ("sp", lambda e: e.dma_start(out=kwaT[96:100, :], in_=CD["kaug"][32:36, :])),
           ("sp", lambda e: e.dma_start(out=kcaT[96:100, :], in_=CD["kcaug"][32:36, :])),
           ("sp", lambda e: e.dma_start(out=ohcol[64:76, :], in_=CD["oh12"].rearrange("a (b c) -> (a b) c", c=12))),
           ("sp", lambda e: e.dma_start(out=sel12p[64:76, :], in_=CD["selpair"].rearrange("a b c -> a (b c)"))),
           ]
    P.dma("sp", fns, writes=[b_c2])
    b_c2p = Buf("consts2p")
    P.dma("pool", [("pool", lambda e: e.dma_start(out=cw1[:, :, :], in_=cw1_d)),
                   ("pool", lambda e: e.dma_start(out=cw2[:, :], in_=cw2_d)),
                   ("pool", lambda e: e.dma_start(out=cposT[:, :], in_=cpos_d))], writes=[b_c2p])
    MS("pool", kwaT[64:96, :], 0.0, PW=[b_kwa])
    MS("pool", kcaT[64:96, :], 0.0, PW=[b_kca])
    MS("pool", kcaT[0:64, :], 0.0, PW=[b_kca])
    MS("pool", vcaug[:, :], 1.0, W=[b_vca])
    MS("pool", vsw[:, :, :, :], 1.0, W=b_vsw)
    MS("pool", selb4[:, :, :], 0.0, W=b_selb4)
    for r in range(4):
        MS("pool", qaT[r][64:96, :], 0.0, W=[b_qsel[r]])
    (S0_, S1_, AC0_, AC1_, D_, S2_, M_, I_) = range(8)
    F_ = I_
    projb[:] = [S0_, S1_, AC0_, AC1_]

    def proj_fm4(wv, c0, ncols, tok0, ntok, bslot, tiles, prow=0):
        psi = projb[pj[0] % 4]
        pj[0] += 1
        for kc in range(16):
            MM(PS[psi][prow:prow + ncols, 0:ntok], wv[:, kc, c0:c0 + ncols], hT[:, kc, tok0:tok0 + ntok],
               start=(kc == 0), stop=(kc == 15), R=[bslot] + [b_hT[t] for t in tiles],
               W=[bPS[psi]] if kc == 0 else (), PW=() if kc == 0 else [bPS[psi]])
        return psi

    for l in range(32):
        MM(PS[S0_][0:64, 0:1], cw1[0:64, l, :], cposT[0:64, l:l + 1], start=(l == 0), stop=(l == 31),
           R=[b_c2p], W=[bPS[S0_]] if l == 0 else (), PW=() if l == 0 else [bPS[S0_]])
        MM(PS[S1_][64:128, 0:1], cw1[64:128, l, :], cposT[64:128, l:l + 1], start=(l == 0), stop=(l == 31),
           R=[b_c2p], W=[bPS[S1_]] if l == 0 else (), PW=() if l == 0 else [bPS[S1_]])
    CP("act", cbias[0:64, :], PS[S0_][0:64, 0:1], R=[bPS[S0_]], PW=[b_cbias])
    CP("act", cbias[64:128, :], PS[S1_][64:128, 0:1], R=[bPS[S1_]], PW=[b_cbias])
    kvc_v = kvcT[:, :].rearrange("p (n s) -> p s n", s=16)
    GC = 2.0 * 0.7978845608028654

    pt_i = [0]
    sc_i = [0]
    acc_i = [0]
    den_i = [0]
    tm_i = [0]
    fb_i = [0]

    for g in range(4):
        slotA, bA = ring_next()
        svA = v16(slotA)
        P.dma("pool", [("pool", lambda e, g=g: e.dma_start(out=svA[:, :, 0:256], in_=w_in_v[:, :, OFF_Q + g * 256:OFF_Q + (g + 1) * 256])),
                       ("pool", lambda e, g=g: e.dma_start(out=svA[:, :, 256:268], in_=w_in_v[:, :, OFF_G + g * 12:OFF_G + (g + 1) * 12]))],
              writes=[bA])
        slotB, bB = ring_next()
        svB = v16(slotB)
        order = [0, 1, 2, 4, 3, 5]
        P.dma("pool", [("pool", lambda e, g=g, i=i, br=br: e.dma_start(
            out=svB[:, :, i * 64:(i + 1) * 64],
            in_=w_in_v[:, :, OFF_KV + br * 256 + g * 64:OFF_KV + br * 256 + (g + 1) * 64])) for i, br in enumerate(order)],
              writes=[bB])
        P.dma("sp", [("sp", lambda e, g=g, r=r: e.dma_start(out=qaT[r][96:100, :], in_=CD["qaug"][4 * g + r, :, :])) for r in range(4)],
              pwrites=b_qa)
        for pair in range(2):
            for c in range(2):
                psi = proj_fm4(svA, pair * 128, 128, 1024 + c * 512, 512, bA, range(8 + 4 * c, 12 + 4 * c))
                for rr in range(2):
                    r = 2 * pair + rr
                    P.op("act", lambda e, r=r, rr=rr, c=c, psi=psi: e.mul(qaT[r][0:64, c * 512:(c + 1) * 512],
                                                                          PS[psi][rr * 64:(rr + 1) * 64, 0:512], 0.125),
                         [bPS[psi]], (), [b_qa[r]])
        for c in range(2):
            psi = proj_fm4(svA, 256, 12, 1024 + c * 512, 512, bA, range(8 + 4 * c, 12 + 4 * c), prow=64)
            ACT(gT[64:76, c * 512:(c + 1) * 512], PS[psi][64:76, 0:512], AF.Sigmoid, R=[bPS[psi]], PW=[b_gT])
        for cj in range(4):
            tiles = range(4 * cj, 4 * cj + 4)
            psi = proj_fm4(svB, 0, 128, cj * 512, 512, bB, tiles)
            CP("dve", kvcT[:, cj * 512:(cj + 1) * 512], PS[psi][:, 0:512], R=[bPS[psi]], PW=[b_kvc])
            psi = proj_fm4(svB, 128, 128, cj * 512, 512, bB, tiles)
            CP("act", ksaT[0:64, cj * 512:(cj + 1) * 512], PS[psi][0:64, 0:512], R=[bPS[psi]], PW=[b_ksa])
            CP("act", kwaT[0:64, cj * 512:(cj + 1) * 512], PS[psi][64:128, 0:512], R=[bPS[psi]], PW=[b_kwa])
        for t in range(16):
            psi = projb[pj[0] % 4]
            pj[0] += 1
            for kc in range(16):
                MM(PS[psi][:, 0:128], hT[:, kc, t * 128:(t + 1) * 128], svB[:, kc, 256:384],
                   start=(kc == 0), stop=(kc == 15), R=[bB, b_hT[t]],
                   W=[bPS[psi]] if kc == 0 else (), PW=() if kc == 0 else [bPS[psi]])
            CP("act" if t % 2 else "dve", vsw[:, t, :, 0:64], PS[psi][:, 0:128].rearrange("p (a b) -> p a b", a=2),
               R=[bPS[psi]], PW=[b_vsw[t]])
        for l in range(32):
            MM(PS[S0_][0:64, 0:127], cw1[0:64, l, :], kvc_v[0:64, l % 16, l // 16:l // 16 + 127],
               start=(l == 0), stop=(l == 31), R=[b_c2p, b_kvc], W=[bPS[S0_]] if l == 0 else (), PW=() if l == 0 else [bPS[S0_]])
            MM(PS[S1_][64:128, 0:127], cw1[64:128, l, :], kvc_v[64:128, l % 16, l // 16:l // 16 + 127],
               start=(l == 0), stop=(l == 31), R=[b_c2p, b_kvc], W=[bPS[S1_]] if l == 0 else (), PW=() if l == 0 else [bPS[S1_]])
        ACT(gu[0:64, 0:127], PS[S0_][0:64, 0:127], AF.Identity, R=[bPS[S0_], b_cbias], W=[b_gu], bias=cbias[0:64, 0:1])
        ACT(gu[64:128, 0:127], PS[S1_][64:128, 0:127], AF.Identity, R=[bPS[S1_], b_cbias], PW=[b_gu], bias=cbias[64:128, 0:1])
        ACT(gq[:, 0:127], gu[:, 0:127], AF.Square, R=[b_gu], W=[b_gq])
        TS("dve", gq[:, 0:127], gq[:, 0:127], 0.044715, 1.0, ALU.mult, ALU.add, R=[b_gq], W=[b_gq])
        TT("dve", gq[:, 0:127], gq[:, 0:127], gu[:, 0:127], ALU.mult, R=[b_gq, b_gu], W=[b_gq])
        ACT(gq[:, 0:127], gq[:, 0:127], AF.Sigmoid, R=[b_gq], W=[b_gq], scale=GC)
        TT("dve", gl[:, 0:127], gq[:, 0:127], gu[:, 0:127], ALU.mult, R=[b_gq, b_gu], W=[b_gl])
        MM(PS[S0_][0:64, 0:127], cw2[0:64, :], gl[0:64, 0:127], R=[b_c2p, b_gl], W=[bPS[S0_]])
        MM(PS[S1_][0:127, 0:64], gl[64:128, 0:127], cw2[64:128, :], R=[b_c2p, b_gl], W=[bPS[S1_]])
        CP("act", kcaT[0:64, 0:127], PS[S0_][0:64, 0:127], R=[bPS[S0_]], W=[b_kca])
        CP("dve", vcaug[0:127, 0:64], PS[S1_][0:127, 0:64], R=[bPS[S1_]], PW=[b_vca])
        if "cmpkv" in debug and g == 0:
            o = dout("dbg_kca", [64, 128], BF16)
            DMA("sp", o, kcaT[0:64, :], R=[b_kca], W=[Buf("dbg_kca")])
            o = dout("dbg_vca", [128, 65], BF16)
            DMA("sp", o, vcaug[:, :], R=[b_vca], W=[Buf("dbg_vca")])

        def finalize(ai, r, br, first, last):
            idx = r * 3 + br
            k6 = (r // 2) * 3 + br
            hp = (r % 2) * 64
            CP("act", numb[hp:hp + 64, k6, :], PS[ai][0:64, :], R=[bPS[ai]], PW=[b_numb[k6]])
            if first:
                TS("dve", dacc[64:76, :], PS[ai][64:76, :], ohcol[64:76, idx:idx + 1], None, ALU.mult,
                   R=[bPS[ai], b_c2], W=[b_dacc])
            else:
                STT(dacc[64:76, :], PS[ai][64:76, :], ohcol[64:76, idx:idx + 1], dacc[64:76, :], ALU.mult, ALU.add,
                    R=[bPS[ai], b_c2, b_dacc], W=[b_dacc])

        def branch(r, br, ktiles, c):
            ai = [AC0_, AC1_][acc_i[0] % 2]
            acc_i[0] += 1
            n = len(ktiles)
            pend = None
            for i, (kap, kb, bias, vap, vb, K, qlo, qhi, blo, bhi) in enumerate(ktiles):
                si = [S0_, S1_, S2_][sc_i[0] % 3]
                sc_i[0] += 1
                MM(PS[si][0:K, qlo:qhi], kap, qaT[r][0:100, c * 512 + qlo:c * 512 + qhi], start=True, stop=(bias is None),
                   R=kb + [b_qa[r], b_qsel[r], b_c2], W=[bPS[si]])
                if bias is not None:
                    MM(PS[si][0:K, blo:bhi], C["ident_b"][0:K, 0:K], bias[0:K, blo:bhi], start=False, stop=True,
                       R=[b_const, b_c2], PW=[bPS[si]])
                if pend is not None:
                    pend()
                pi = pt_i[0] % NPT
                pt_i[0] += 1
                ACT(pTb[pi][0:K, qlo:qhi], PS[si][0:K, qlo:qhi], AF.Exp, R=[bPS[si]], W=[b_pT[pi]])

                def pv(i=i, pi=pi, vap=vap, vb=vb, K=K, qlo=qlo, qhi=qhi):
                    MM(PS[ai][0:76, qlo:qhi], vap, pTb[pi][0:K, qlo:qhi], start=(i == 0), stop=(i == n - 1),
                       R=vb + [b_pT[pi]], W=[bPS[ai]] if i == 0 else (), PW=() if i == 0 else [bPS[ai]])
                pend = pv
                if br == 0:
                    pend()
                    pend = None
                    ib = [I_, D_][r % 2]
                    for qt in range(4):
                        MM(PS[ib][:, qt * 64:qt * 64 + 33], pTb[pi][0:127, qt * 128:(qt + 1) * 128], C["ovaug"][0:127, 0:33],
                           R=[b_pT[pi], b_const], W=[bPS[ib]] if qt == 0 else (), PW=() if qt == 0 else [bPS[ib]])
                    for qt in range(4):
                        TS("dve", sm[:, qt, 0:1], PS[ib][:, qt * 64 + 32:qt * 64 + 33], 1e-30, None, ALU.max,
                           R=[bPS[ib]], W=[b_sm[qt]])
                        RECIP(sm[:, qt, 1:2], sm[:, qt, 0:1], R=[b_sm[qt]], PW=[b_sm[qt]])
                        if r == 0:
                            TS("dve", impt[:, qt, :], PS[ib][:, qt * 64:qt * 64 + 32], sm[:, qt, 1:2], None, ALU.mult,
                               R=[bPS[ib], b_sm[qt]], W=[b_imp[qt]])
                        else:
                            STT(impt[:, qt, :], PS[ib][:, qt * 64:qt * 64 + 32], sm[:, qt, 1:2], impt[:, qt, :], ALU.mult, ALU.add,
                                R=[bPS[ib], b_sm[qt], b_imp[qt]], W=[b_imp[qt]])
            if pend is not None:
                pend()
            return ai

        for c in range(2):
            q0 = 8 + 4 * c
            nfin = [0]
            for r in range(4):
                ai = branch(r, 0, [(kcaT[0:100, 0:127], [b_kca], cbc[:, c, :], vcaug[0:127, 0:76], [b_vca], 127, 0, 512, 0, 512)], c)
                finalize(ai, r, 0, nfin[0] == 0, False)
                nfin[0] += 1
            for qt in range(4):
                qi = 4 * c + qt
                TT("dve", imp2[:, :], impt[:, qt, :], selF[:, qi, :], ALU.max, R=[b_imp[qt], b_c2], W=[b_sel])
                TT("dve", imp2[:, :], imp2[:, :], selU[:, qi, :], ALU.min, R=[b_c2], W=[b_sel])
                P.op("dve", lambda e: e.max(m8a[:, :], imp2[:, :]), [], [b_sel])
                P.op("dve", lambda e: e.match_replace(imp3[:, :], m8a[:, :], imp2[:, :], -3.0e38), [], [b_sel])
                P.op("dve", lambda e: e.max(m8b[:, :], imp3[:, :]), [], [b_sel])
                TS("dve", m8b[:, 7:8], m8b[:, 7:8], -1.0e29, None, ALU.max, W=[b_sel])
                TS("dve", selb4[:, qt, 64:96], imp2[:, :], m8b[:, 7:8], NEGB, ALU.is_lt, ALU.mult, R=[b_sel], W=[b_selb4[qt]])
            for r in range(4):
                kt_list = []
                for o in (-1, -2, -3, -4, 0, 1, 2, 3):
                    kt = q0 + o
                    if o < 0:
                        m_ = o + 4
                        rng_ = (0, 128 * (m_ + 1), 128 * m_, 128 * (m_ + 1))
                    else:
                        rng_ = (128 * o, 512, 128 * o, 128 * (o + 1))
                    kt_list.append((kwaT[0:100, kt * 128:(kt + 1) * 128], [b_kwa], cb[:, o + 4, :], vsw[:, kt, 1, :], [b_vsw[kt]], 128) + rng_)
                ai = branch(r, 2, kt_list, c)
                finalize(ai, r, 2, False, False)
                nfin[0] += 1
            for qt in range(4):
                pv_ = PS[I_][:, :].bitcast(BF16)
                TR(pv_[:, 0:128], selb4[:, qt, :], C["ident_b"][:], R=[b_selb4[qt], b_const], W=[bPS[I_]])
                for r in range(4):
                    CP("act", qaT[r][64:96, c * 512 + qt * 128:c * 512 + (qt + 1) * 128], pv_[64:96, 0:128],
                       R=[bPS[I_]], PW=[b_qsel[r]])
            for r in range(4):
                kt_list = []
                for kt in range(0, q0 + 4):
                    o = kt - q0
                    rng_ = (128 * o, 512, 128 * o, 128 * (o + 1)) if o >= 0 else (0, 512, 0, 512)
                    kt_list.append((ksaT[0:100, kt * 128:(kt + 1) * 128], [b_ksa], cb[:, 4 + o, :] if o >= 0 else None,
                                    vsw[:, kt, 0, :], [b_vsw[kt]], 128) + rng_)
                ai = branch(r, 1, kt_list, c)
                finalize(ai, r, 1, False, nfin[0] == 11)
                nfin[0] += 1
            TS("dve", Dsb[64:76, :], dacc[64:76, :], 1e-30, None, ALU.max, R=[b_dacc], W=[b_Dsb])
            RECIP(Dsb[64:76, :], Dsb[64:76, :], R=[b_Dsb], W=[b_Dsb])
            TT("dve", fT[64:76, :], Dsb[64:76, :], gT[64:76, c * 512:(c + 1) * 512], ALU.mult, R=[b_Dsb, b_gT], W=[b_fT])
            for pair in range(2):
                for br in range(3):
                    k6 = pair * 3 + br
                    fb = [I_, D_][fb_i[0] % 2]
                    fb_i[0] += 1
                    MM(PS[fb][:, :], sel12p[64:76, k6 * 128:(k6 + 1) * 128], fT[64:76, :], R=[b_c2, b_fT], W=[bPS[fb]])
                    ti = tm_i[0] % 2
                    tm_i[0] += 1
                    TT("dve", tmul[ti][:, :], numb[:, k6, :], PS[fb][:, :], ALU.mult, R=[b_numb[k6], bPS[fb]], W=[b_tmul[ti]])
                    MM(PS[M_][:, :], C["ident_b"][:, :], tmul[ti][:, :], start=(br == 0), stop=(br == 2),
                       R=[b_const, b_tmul[ti]], W=[bPS[M_]] if br == 0 else (), PW=() if br == 0 else [bPS[M_]])
                mc = 2 * g + pair
                CP("act", mixT[:, mc, c * 512:(c + 1) * 512], PS[M_][:, :], R=[bPS[M_]], PW=[b_mix[mc]])
    print("SBUF KiB used end of NSA", (A.cur - A.lo) / 1024.0)
    if "mixnsa" in debug:
        o = dout("dbg_mixnsa", [128, 8, NOWN], BF16)
        bd = Buf("dbg_mixnsa")
        for kc in range(8):
            DMA("sp", o[:, kc, :], mixT[:, kc, :], R=b_mix[0:8], PW=[bd])
    P.barrier()
    A.release(m_big)
    if stage <= 3:
        P.finish()
        P.replay()
        return nc, OUTS

    w_out = din("w_out", [D, D])
    w_out_v = w_out.rearrange("(kc p) n -> p kc n", p=128)
    n2w_d = din("n2w_row", [1, D])
    nfw_d = din("nfw_row", [1, D])
    wr_d = din("wr", [D, 36])
    br_d = din("br", [1, 36])

    x1 = A.alloc("x1", [128, 8, D], F32)
    b_x1 = [Buf("x1_%d" % i) for i in range(8)]
    m_p3 = A.mark()
    for i in range(8):
        DMA("sp", x1[:, i, :], xctx[1024 + i * 128:1024 + (i + 1) * 128, :], W=[b_x1[i]])
    g1bc = A.alloc("g1bc", [128, D], F32)
    b_g1 = Buf("g1bc")
    dgt[0] = None
    ada_rebuild(0, g1bc, b_g1)
    ytmp = [A.alloc("ytmp%d" % i, [128, 512], F32) for i in range(2)]
    b_yt = [Buf("ytmp%d" % i) for i in range(2)]
    n3 = 0
    for j in range(4):
        slot, bslot = ring_next()
        sv = v16(slot)
        DMA("pool", sv, w_out_v[:, :, j * 512:(j + 1) * 512], W=[bslot])
        for i in range(8):
            psi = 2 + n3 % 2
            yi = n3 % 2
            n3 += 1
            for kc in range(16):
                MM(PS[psi][:, :], mixT[:, kc, i * 128:(i + 1) * 128], sv[:, kc, :], start=(kc == 0), stop=(kc == 15),
                   R=[bslot, b_mix[kc]], W=[bPS[psi]] if kc == 0 else (), PW=() if kc == 0 else [bPS[psi]])
            TT("dve", ytmp[yi][:, :], PS[psi][:, :], g1bc[:, j * 512:(j + 1) * 512], ALU.mult, R=[bPS[psi], b_g1], W=[b_yt[yi]])
            TT("dve", x1[:, i, j * 512:(j + 1) * 512], ytmp[yi][:, :], x1[:, i, j * 512:(j + 1) * 512], ALU.add,
               R=[b_yt[yi], b_x1[i]], W=[b_x1[i]])
    if "x1" in debug:
        o = dout("dbg_x1", [128, 8, D], F32)
        bd = Buf("dbg_x1")
        for i in range(8):
            DMA("sp", o[:, i, :], x1[:, i, :], R=[b_x1[i]], PW=[bd])
    P.barrier()
    A.release(m_p3)
    if stage <= 4:
        P.finish()
        P.replay()
        return nc, OUTS

    h2b = A.alloc_at("h2b", [128, 8, D], BF16, off_mix)
    b_h2b = [Buf("h2b%d" % i) for i in range(8)]
    g2bc = A.alloc("g2bc", [128, D], F32)
    b_g2 = Buf("g2bc")
    Wc = A.alloc("Wc", [128, 8, 32], F32)
    posm = A.alloc("posm", [128, 8, 32], F32)
    selm = A.alloc("selm", [128, 8, 32], F32)
    selmb = A.alloc("selmb", [128, 8, 32], BF16)
    b_rt = [Buf("rt%d" % i) for i in range(8)]
    b_selmb = Buf("selmb")
    b_posm = [Buf("posm%d" % i) for i in range(8)]
    st2 = A.alloc("st2", [128, 8, 4], F32)
    b_st2 = [Buf("st2_%d" % i) for i in range(8)]
    m_p4 = A.mark()
    A2bc = A.alloc("A2bc", [128, D], F32)
    sh2bc = A.alloc("sh2bc", [128, D], F32)
    n2wbc = A.alloc("n2wbc", [128, D], F32)
    b_A2, b_sh2, b_n2w = Buf("A2"), Buf("sh2"), Buf("n2w")
    DMA("sp", n2wbc[:, :], n2w_d.partition_broadcast(128), W=[b_n2w])
    dgt[0] = None
    ada_rebuild(1, sh2bc, b_sh2)
    ada_rebuild(2, A2bc, b_A2)
    ada_rebuild(3, g2bc, b_g2)
    STT(A2bc[:, :], A2bc[:, :], 1.0, n2wbc[:, :], ALU.add, ALU.mult, R=[b_A2, b_n2w], W=[b_A2])
    h2f0 = A.alloc("h2f", [128, D], F32)
    h2fs = [h2f0, n2wbc]
    b_h2fs = [Buf("h2f"), b_n2w]
    h2T = A.alloc("h2T", [128, 16, 128], F32)
    b_h2T = Buf("h2T")
    wr = A.alloc("wr", [128, 16, 36], F32)
    brt = A.alloc("brt", [1, 36], F32)
    b_wr = Buf("wr")
    P.dma("sp", [("sp", lambda e: e.dma_start(out=wr[:, :, :], in_=wr_d.rearrange("(kc p) n -> p kc n", p=128))),
                 ("sp", lambda e: e.dma_start(out=brt[0:1, :], in_=br_d))], writes=[b_wr])
    rsets = []
    for k_ in range(2):
        rsets.append((A.alloc("lg", [128, 36], F32), A.alloc("rs", [128, 16], F32), A.alloc("gm", [128, 4], F32),
                      A.alloc("msk", [128, 32], F32), A.alloc("m8r", [128, 8], F32), A.alloc("s1w", [128, 32], F32),
                      A.alloc("s2w", [128, 32], F32), Buf("routescratch%d" % k_)))
    def p4_norm(i):
        h2f, b_h2f = h2fs[i % 2], b_h2fs[i % 2]
        ACT(h2b[:, i, :], x1[:, i, :], AF.Square, R=[b_x1[i]], W=[b_h2b[i], b_st2[i]], accum=st2[:, i, 0:1])
        ACT(st2[:, i, 1:2], st2[:, i, 0:1], AF.Sqrt, R=[b_st2[i]], PW=[b_st2[i]], scale=1.0 / D, bias=EPS)
        RECIP(st2[:, i, 2:3], st2[:, i, 1:2], R=[b_st2[i]], PW=[b_st2[i]])
        STT(h2f[:, :], x1[:, i, :], st2[:, i, 2:3], A2bc[:, :], ALU.mult, ALU.mult, R=[b_x1[i], b_st2[i], b_A2], W=[b_h2f])
        TT("dve", h2f[:, :], h2f[:, :], sh2bc[:, :], ALU.add, R=[b_h2f, b_sh2], W=[b_h2f])
        CP("act", h2b[:, i, :], h2f[:, :], R=[b_h2f], W=[b_h2b[i]])

    def p4_pe(i):
        h2f, b_h2f = h2fs[i % 2], b_h2fs[i % 2]
        for kg in range(4):
            psi = kg % 2
            for k4 in range(4):
                kc = 4 * kg + k4
                TR(PS[psi][:, k4 * 128:(k4 + 1) * 128], h2f[:, kc * 128:(kc + 1) * 128], C["ident_f"][:],
                   R=[b_h2f, b_const], W=[bPS[psi]] if k4 == 0 else (), PW=() if k4 == 0 else [bPS[psi]])
            CP("act", h2T[:, 4 * kg:4 * kg + 4, :], PS[psi][:, :].rearrange("p (a b) -> p a b", a=4),
               R=[bPS[psi]], W=[b_h2T] if kg == 0 else (), PW=() if kg == 0 else [b_h2T])
        for kc in range(16):
            MM(PS[2][:, 0:36], h2T[:, kc, :], wr[:, kc, :], start=(kc == 0), stop=False, R=[b_h2T, b_wr],
               W=[bPS[2]] if kc == 0 else (), PW=() if kc == 0 else [bPS[2]])
        MM(PS[2][:, 0:36], C["ones_f"][0:1, :], brt[0:1, :], start=False, stop=True, R=[b_const, b_wr], PW=[bPS[2]])

    def p4_route(i):
        lg, rs, gm, msk, m8, s1w, s2w, b_r = rsets[i % 2]
        CP("dve", lg[:, :], PS[2][:, 0:36], R=[bPS[2]], W=[b_r])
        P.op("dve", lambda e, rs=rs, lg=lg: e.tensor_reduce(rs[:, 0:1], lg[:, 0:4], AX.X, ALU.max), [], [b_r])
        TS("dve", rs[:, 1:2], rs[:, 0:1], -1.0, None, ALU.mult, W=[b_r])
        ACT(gm[:, :], lg[:, 0:4], AF.Exp, R=[b_r], W=[b_r], bias=rs[:, 1:2], accum=rs[:, 2:3])
        RECIP(rs[:, 3:4], rs[:, 2:3], W=[b_r])
        TS("dve", gm[:, :], lg[:, 0:4], rs[:, 0:1], None, ALU.is_equal, W=[b_r])
        TS("dve", gm[:, :], gm[:, :], -1.0, 1.0e30, ALU.add, ALU.mult, W=[b_r])
        TT("dve", msk[:, :].rearrange("p (a b) -> p a b", a=4), lg[:, 4:36].rearrange("p (a b) -> p a b", a=4),
           gm[:, :].unsqueeze(2).broadcast_to([128, 4, 8]), ALU.add, W=[b_r])
        P.op("dve", lambda e, m8=m8, msk=msk: e.max(m8[:, :], msk[:, :]), [], [b_r])
        TT("dve", rs[:, 4:5], m8[:, 1:2], m8[:, 0:1], ALU.subtract, W=[b_r])
        ACT(rs[:, 5:6], rs[:, 4:5], AF.Exp, R=[b_r], W=[b_r])
        TS("dve", rs[:, 6:7], rs[:, 5:6], 1.0, None, ALU.add, W=[b_r])
        RECIP(rs[:, 7:8], rs[:, 6:7], W=[b_r])
        TT("dve", rs[:, 8:9], rs[:, 7:8], rs[:, 3:4], ALU.mult, W=[b_r])
        TT("dve", rs[:, 9:10], rs[:, 5:6], rs[:, 8:9], ALU.mult, W=[b_r])
        TS("dve", s1w[:, :], msk[:, :], m8[:, 0:1], rs[:, 8:9], ALU.is_equal, ALU.mult, W=[b_r])
        TS("dve", s2w[:, :], msk[:, :], m8[:, 1:2], rs[:, 9:10], ALU.is_equal, ALU.mult, W=[b_r])
        TT("dve", Wc[:, i, :], s1w[:, :], s2w[:, :], ALU.add, R=[b_r], W=[b_rt[i]])
        TS("dve", selm[:, i, :], msk[:, :], m8[:, 1:2], None, ALU.is_ge, R=[b_r], PW=[b_rt[i]])

    p4_norm(0)
    for i in range(8):
        p4_pe(i)
        if i + 1 < 8:
            p4_norm(i + 1)
        p4_route(i)
    CP("dve", selmb[:, :, :], selm[:, :, :], R=b_rt, W=[b_selmb])
    for i in range(8):
        sb0 = 4 * (i // 4)
        prev = list(range(sb0, i))
        MM(PS[3][:, 0:32], C["lstrict"][:, :], selmb[:, i, :], start=True, stop=(len(prev) == 0),
           R=[b_const, b_selmb], W=[bPS[3]])
        for n_, ip in enumerate(prev):
            MM(PS[3][:, 0:32], C["ones_b"][:, :], selmb[:, ip, :], start=False, stop=(n_ == len(prev) - 1),
               R=[b_const, b_selmb], PW=[bPS[3]])
        STT(posm[:, i, :], PS[3][:, 0:32], 1.0, selm[:, i, :], ALU.add, ALU.mult, R=[bPS[3], b_rt[i]], W=[b_posm[i]])
        TS("dve", posm[:, i, :], posm[:, i, :], -1.0, None, ALU.add, R=[b_posm[i]], W=[b_posm[i]])
    if "route" in debug:
        o = dout("dbg_Wc", [128, 8, 32], F32)
        DMA("sp", o, Wc[:, :, :], R=b_rt, W=[Buf("dbg_Wc")])
        o = dout("dbg_posm", [128, 8, 32], F32)
        DMA("sp", o, posm[:, :, :], R=b_posm, W=[Buf("dbg_posm")])
        o = dout("dbg_h2b", [128, 8, D], BF16)
        bd = Buf("dbg_h2b")
        for i in range(8):
            DMA("sp", o[:, i, :], h2b[:, i, :], R=[b_h2b[i]], PW=[bd])
    P.barrier()
    A.release(m_p4)
    if stage <= 5:
        P.finish()
        P.replay()
        return nc, OUTS

    weg = din("w_expert_gate", [32, D, 512])
    weu = din("w_expert_up", [32, D, 512])
    wed = din("w_expert_down", [32, 512, D])
    out_d = dout("out", [NOWN, D])
    ring.append(A.alloc("ring2", [128, 8192], BF16))
    bring.append(Buf("ring2"))
    NRING3 = 3

    def ring_next3():
        i = rstate[0] % NRING3
        rstate[0] += 1
        return ring[i], bring[i]

    PmS = [[A.alloc("Pm%d%d" % (p_, i), [128, 4, 128], BF16) for i in range(2)] for p_ in range(2)]
    PwS = [[A.alloc("Pw%d%d" % (p_, i), [128, 4, 128], BF16) for i in range(2)] for p_ in range(2)]
    b_PmS = [[Buf("Pm%d%d" % (p_, i)) for i in range(2)] for p_ in range(2)]
    b_PwS = [[Buf("Pw%d%d" % (p_, i)) for i in range(2)] for p_ in range(2)]

    def make_P(ex):
        Pm, Pw, b_Pm, b_Pw = PmS[ex % 2], PwS[ex % 2], b_PmS[ex % 2], b_PwS[ex % 2]
        for sb in range(2):
            for il in range(4):
                i = 4 * sb + il
                TS("dve", Pm[sb][:, il, :], C["iota"][:, :], posm[:, i, ex:ex + 1], None, ALU.is_equal,
                   R=[b_const, b_posm[i]], W=[b_Pm[sb]] if il == 0 else (), PW=() if il == 0 else [b_Pm[sb]])
                TS("dve", Pw[sb][:, il, :], C["iota"][:, :], posm[:, i, ex:ex + 1], Wc[:, i, ex:ex + 1], ALU.is_equal, ALU.mult,
                   R=[b_const, b_posm[i], b_rt[i]], W=[b_Pw[sb]] if il == 0 else (), PW=() if il == 0 else [b_Pw[sb]])

    make_P(0)
    xeT = [A.alloc("xeT%d" % i, [128, 16, 128], BF16) for i in range(2)]
    b_xe = [Buf("xeT%d" % i) for i in range(2)]
    PwT = A.alloc("PwT", [128, 2, 2, 512], BF16)
    b_PwT = [[Buf("PwT%d%d" % (a_, b_)) for b_ in range(2)] for a_ in range(2)]
    sg = [A.alloc("sg%d" % i, [128, 512], F32) for i in range(2)]
    b_sg = [Buf("sg%d" % i) for i in range(2)]
    actT = [A.alloc("actT%d" % i, [128, 4, 128], BF16) for i in range(2)]
    b_act = [Buf("actT%d" % i) for i in range(2)]
    ye = A.alloc("ye", [128, 2, 2, D], BF16)
    b_ye = [[Buf("ye%d%d" % (a_, b_)) for b_ in range(2)] for a_ in range(2)]
    (DA_, DB_, G0_, U0_, G1_, U1_, YY_, CC_) = range(8)
    GB = [(G0_, U0_), (G1_, U1_)]
    nd = [0]
    ny = [0]
    for e2 in range(16):
        for el in range(2):
            ex = 2 * e2 + el
            sg_, bg_ = ring_next3()
            wgv = v16(sg_)
            DMA("pool", wgv, weg[ex].rearrange("(kc p) n -> p kc n", p=128), W=[bg_])
            su_, bu_ = ring_next3()
            wuv = v16(su_)
            DMA("pool", wuv, weu[ex].rearrange("(kc p) n -> p kc n", p=128), W=[bu_])
            sd_, bd_ = ring_next3()
            wdv = sd_[:, :].rearrange("p (a b) -> p a b", a=4)
            DMA("pool", wdv, wed[ex].rearrange("(fc p) n -> p fc n", p=128), W=[bd_])
            Pm, Pw, b_Pm, b_Pw = PmS[ex % 2], PwS[ex % 2], b_PmS[ex % 2], b_PwS[ex % 2]
            for sb in range(2):
                for kg in range(4):
                    psi = [DA_, DB_][nd[0] % 2]
                    nd[0] += 1
                    for k4 in range(4):
                        kc = 4 * kg + k4
                        for il in range(4):
                            i = 4 * sb + il
                            MM(PS[psi][:, k4 * 128:(k4 + 1) * 128], h2b[:, i, kc * 128:(kc + 1) * 128], Pm[sb][:, il, :],
                               start=(il == 0), stop=(il == 3), R=[b_h2b[i], b_Pm[sb]],
                               W=[bPS[psi]] if (k4 == 0 and il == 0) else (), PW=() if (k4 == 0 and il == 0) else [bPS[psi]])
                    CP("act" if kg % 2 else "dve", xeT[sb][:, 4 * kg:4 * kg + 4, :], PS[psi][:, :].rearrange("p (a b) -> p a b", a=4),
                       R=[bPS[psi]], W=[b_xe[sb]] if kg == 0 else (), PW=() if kg == 0 else [b_xe[sb]])
            for sb in range(2):
                psi = [DA_, DB_][nd[0] % 2]
                nd[0] += 1
                pvT = PS[psi][:, :].bitcast(BF16)
                for il in range(4):
                    TR(pvT[:, il * 128:(il + 1) * 128], Pw[sb][:, il, :], C["ident_b"][:], R=[b_Pw[sb], b_const],
                       W=[bPS[psi]] if il == 0 else (), PW=() if il == 0 else [bPS[psi]])
                CP("act", PwT[:, sb, el, :], pvT[:, 0:512], R=[bPS[psi]], W=[b_PwT[sb][el]])
            if ex + 1 < 32:
                make_P(ex + 1)
            for sb in range(2):
                for (wv, bw, psi) in ((wgv, bg_, GB[sb][0]), (wuv, bu_, GB[sb][1])):
                    for fc in range(4):
                        for kc in range(16):
                            MM(PS[psi][:, fc * 128:(fc + 1) * 128], wv[:, kc, fc * 128:(fc + 1) * 128], xeT[sb][:, kc, :],
                               start=(kc == 0), stop=(kc == 15), R=[bw, b_xe[sb]],
                               W=[bPS[psi]] if (fc == 0 and kc == 0) else (), PW=() if (fc == 0 and kc == 0) else [bPS[psi]])
                ACT(sg[sb][:, :], PS[GB[sb][0]][:, :], AF.Silu, R=[bPS[GB[sb][0]]], W=[b_sg[sb]])
                TT("dve", actT[sb][:, :, :].rearrange("p a b -> p (a b)"), sg[sb][:, :], PS[GB[sb][1]][:, :], ALU.mult,
                   R=[b_sg[sb], bPS[GB[sb][1]]], W=[b_act[sb]])
            for sb in range(2):
                for nb in range(4):
                    psi = [YY_, CC_][ny[0] % 2]
                    ny[0] += 1
                    for fc in range(4):
                        MM(PS[psi][:, :], actT[sb][:, fc, :], wdv[:, fc, nb * 512:(nb + 1) * 512], start=(fc == 0), stop=(fc == 3),
                           R=[b_act[sb], bd_], W=[bPS[psi]] if fc == 0 else (), PW=() if fc == 0 else [bPS[psi]])
                    TT("dve", ye[:, sb, el, nb * 512:(nb + 1) * 512], PS[psi][:, :], g2bc[:, nb * 512:(nb + 1) * 512], ALU.mult,
                       R=[bPS[psi], b_g2], W=[b_ye[sb][el]] if nb == 0 else (), PW=() if nb == 0 else [b_ye[sb][el]])
        for sb in range(2):
            for il in range(4):
                i = 4 * sb + il
                for nb in range(4):
                    psi = [YY_, CC_][ny[0] % 2]
                    ny[0] += 1
                    for el in range(2):
                        MM(PS[psi][:, :], PwT[:, sb, el, il * 128:(il + 1) * 128], ye[:, sb, el, nb * 512:(nb + 1) * 512],
                           start=(el == 0), stop=(el == 1), R=[b_PwT[sb][el], b_ye[sb][el]],
                           W=[bPS[psi]] if el == 0 else (), PW=() if el == 0 else [bPS[psi]])
                    TT("dve", x1[:, i, nb * 512:(nb + 1) * 512], PS[psi][:, :], x1[:, i, nb * 512:(nb + 1) * 512], ALU.add,
                       R=[bPS[psi], b_x1[i]], W=[b_x1[i]])
    print("SBUF KiB used end of MoE", (A.cur - A.lo) / 1024.0)
    P.barrier()
    A.release(m_p4)

    nfbc = A.alloc("nfbc", [128, D], F32)
    b_nf = Buf("nfbc")
    DMA("sp", nfbc[:, :], nfw_d.partition_broadcast(128), W=[b_nf])
    ost = [A.alloc("ost%d" % i, [128, D], F32) for i in range(2)]
    b_ost = [Buf("ost%d" % i) for i in range(2)]
    b_out = Buf("out")
    for i in range(8):
        oi = i % 2
        ACT(ost[oi][:, :], x1[:, i, :], AF.Square, R=[b_x1[i]], W=[b_ost[oi], b_st2[i]], accum=st2[:, i, 0:1])
        ACT(st2[:, i, 1:2], st2[:, i, 0:1], AF.Sqrt, R=[b_st2[i]], PW=[b_st2[i]], scale=1.0 / D, bias=EPS)
        RECIP(st2[:, i, 2:3], st2[:, i, 1:2], R=[b_st2[i]], PW=[b_st2[i]])
        STT(ost[oi][:, :], x1[:, i, :], st2[:, i, 2:3], nfbc[:, :], ALU.mult, ALU.mult, R=[b_x1[i], b_st2[i], b_nf], W=[b_ost[oi]])
        DMA("sp", out_d[i * 128:(i + 1) * 128, :], ost[oi][:, :], R=[b_ost[oi]], PW=[b_out], owner=b_ost[oi])
    print('SBUF peak KiB', (A.peak - A.lo) / 1024.0, 'of', (A.hi - A.lo) / 1024.0)
    P.finish()
    P.replay()
    return nc, OUTS


def core_inputs(inputs, b, s, consts):
    x = np.asarray(inputs["x"], np.float32)
    m = {}
    xc = np.zeros((NCTX, D), np.float32)
    if s == 1:
        xc[:] = x[b]
    else:
        xc[1024:] = x[b, :1024]
    m["xctx"] = xc
    m["cT"] = np.ascontiguousarray(np.asarray(inputs["c"], np.float32)[b].reshape(16, 128).T)
    m["w_ada"] = np.asarray(inputs["w_ada"], np.float32)[0]
    m["b_ada"] = np.ascontiguousarray(np.asarray(inputs["b_ada"], np.float32)[0].reshape(24, 512))
    m["n1w"] = np.ascontiguousarray(np.asarray(inputs["norm1_w"], np.float32)[0].reshape(16, 128).T)
    m["w_in"] = np.asarray(inputs["w_in"], np.float32)[0]
    m["w2aug"] = np.ascontiguousarray(np.concatenate([np.asarray(inputs["gla_w_gate2"], np.float32)[0],
                                                      np.asarray(inputs["gla_b_gate"], np.float32)[0][None, :]], axis=0))
    m["gnw"] = np.ascontiguousarray(np.asarray(inputs["gla_norm_w"], np.float32)[0].reshape(2, 128).T)
    w1k = np.asarray(inputs["cmp_w1_k"], np.float32)[0].reshape(32, 64, 64).transpose(1, 0, 2)
    w1v = np.asarray(inputs["cmp_w1_v"], np.float32)[0].reshape(32, 64, 64).transpose(1, 0, 2)
    m["cw1"] = np.ascontiguousarray(np.concatenate([w1k, w1v], axis=0))
    m["cw2"] = np.ascontiguousarray(np.concatenate([np.asarray(inputs["cmp_w2_k"], np.float32)[0],
                                                    np.asarray(inputs["cmp_w2_v"], np.float32)[0]], axis=0))
    m["cposT"] = np.ascontiguousarray(np.concatenate([np.asarray(inputs["cmp_pos_k"], np.float32)[0].T,
                                                      np.asarray(inputs["cmp_pos_v"], np.float32)[0].T], axis=0))
    m["w_out"] = np.asarray(inputs["w_out"], np.float32)[0]
    m["n2w_row"] = np.asarray(inputs["norm2_w"], np.float32)[0].reshape(1, D)
    m["nfw_row"] = np.asarray(inputs["norm_f_w"], np.float32).reshape(1, D)
    m["wr"] = np.ascontiguousarray(np.concatenate([np.asarray(inputs["w_router_group"], np.float32)[0],
                                                   np.asarray(inputs["w_router_expert"], np.float32)[0]], axis=1))
    m["br"] = np.ascontiguousarray(np.concatenate([np.asarray(inputs["b_router_group"], np.float32)[0],
                                                   np.asarray(inputs["b_router_expert"], np.float32)[0]])[None, :])
    m["w_expert_gate"] = np.asarray(inputs["w_expert_gate"], np.float32)[0]
    m["w_expert_up"] = np.asarray(inputs["w_expert_up"], np.float32)[0]
    m["w_expert_down"] = np.asarray(inputs["w_expert_down"], np.float32)[0]
    for k, v in consts[s].items():
        m["c_" + k] = v
    return m


_CACHE = {}


def kernel(**inputs):
    if "nc" not in _CACHE:
        _CACHE["nc"] = build()[0]
        _CACHE["consts"] = [make_consts(0), make_consts(1)]
    nc = _CACHE["nc"]
    consts = _CACHE["consts"]
    maps = [core_inputs(inputs, c // 2, c % 2, consts) for c in range(8)]
    res = run_bass_kernel_spmd(nc, maps, core_ids=list(range(8)))
    out = np.zeros((4, SEQ, D), np.float32)
    for c in range(8):
        b, sh = c // 2, c % 2
        out[b, sh * 1024:(sh + 1) * 1024] = np.asarray(res.results[c]["out"], np.float32)
    return out
```
